# Optimizing a Trainium2 kernel written in Bass

```python
import math
import jax, jax.numpy as jnp
from jax import lax
import numpy as np

D_MODEL = 2048
BATCH = 4
SEQ = 2048
DEPTH = 1

CHUNK = 64
Q_BLOCK = 128
D_MIX = D_MODEL
DIFF_WIDTH = D_MIX // 2
GLA_WIDTH = D_MIX - DIFF_WIDTH
DIFF_QK_DIM = 64
DIFF_V_DIM = 2 * DIFF_QK_DIM
DIFF_HEADS = DIFF_WIDTH // DIFF_V_DIM
ROT_DIM = DIFF_QK_DIM // 4
ROPE_THETA = 500000.0
GLA_HEADS = 4
GLA_V_DIM = GLA_WIDTH // GLA_HEADS
GLA_K_DIM = GLA_V_DIM // 2
GLA_GATE_RANK = 16
GLA_TAU = 16.0
N_GROUPS = 4
EXPERTS_PER_GROUP = 8
TOP_K = 2
D_FF_EXPERT = D_MODEL // 4
RMS_EPS = 1e-6

IN_SPLITS = (DIFF_HEADS * 2 * DIFF_QK_DIM,
             DIFF_HEADS * 2 * DIFF_QK_DIM,
             DIFF_HEADS * DIFF_V_DIM,
             GLA_HEADS * GLA_K_DIM,
             GLA_HEADS * GLA_K_DIM,
             GLA_HEADS * GLA_V_DIM,
             GLA_HEADS * GLA_V_DIM,
             GLA_GATE_RANK)
D_IN = sum(IN_SPLITS)

kernel_name = "hymba_diffattn_gla_hier_moe_block"


def rms_norm(x, g):
    xf = x.astype(jnp.float32)
    y = xf * lax.rsqrt(jnp.mean(xf * xf, axis=-1, keepdims=True) + RMS_EPS)
    return (y * g.astype(jnp.float32)).astype(x.dtype)


def partial_rope(t, cos, sin):
    half = ROT_DIM // 2
    x1 = t[..., :half].astype(jnp.float32)
    x2 = t[..., half:ROT_DIM].astype(jnp.float32)
    r = jnp.concatenate([x1 * cos - x2 * sin, x2 * cos + x1 * sin], axis=-1).astype(t.dtype)
    return jnp.concatenate([r, t[..., ROT_DIM:]], axis=-1)


def diff_attention(q, k, v, positions, q_g, k_g, lq1, lk1, lq2, lk2, out_g, lambda_init):
    B, S = q.shape[0], q.shape[1]
    q = rms_norm(q, q_g)
    k = rms_norm(k, k_g)
    inv = ROPE_THETA ** (-jnp.arange(0, ROT_DIM, 2, dtype=jnp.float32) / ROT_DIM)
    ang = positions.astype(jnp.float32)[..., None] * inv
    cos = jnp.cos(ang)[:, :, None, None, :]
    sin = jnp.sin(ang)[:, :, None, None, :]
    q = partial_rope(q, cos, sin)
    k = partial_rope(k, cos, sin)
    lam = (jnp.exp(jnp.sum(lq1.astype(jnp.float32) * lk1.astype(jnp.float32)))
           - jnp.exp(jnp.sum(lq2.astype(jnp.float32) * lk2.astype(jnp.float32)))
           + lambda_init)
    scale = DIFF_QK_DIM ** -0.5
    n_qb = S // Q_BLOCK
    qb = q.reshape(B, n_qb, Q_BLOCK, DIFF_HEADS, 2, DIFF_QK_DIM).transpose(1, 0, 3, 4, 2, 5)
    kt = k.transpose(0, 2, 3, 1, 4)
    vt = v.transpose(0, 2, 1, 3)
    key_chunk = jnp.arange(S) // CHUNK

    def block(args):
        qi, idx = args
        s = jnp.einsum('bhmqd,bhmkd->bhmqk', qi, kt, preferred_element_type=jnp.float32) * scale
        q_chunk = (idx * Q_BLOCK + jnp.arange(Q_BLOCK)) // CHUNK
        mask = key_chunk[None, :] <= q_chunk[:, None]
        s = jnp.where(mask, s, -jnp.inf)
        p = jax.nn.softmax(s, axis=-1)
        w = p[:, :, 0] - lam * p[:, :, 1]
        return jnp.einsum('bhqk,bhkd->bhqd', w.astype(vt.dtype), vt)

    o = lax.map(block, (qb, jnp.arange(n_qb)))
    o = o.transpose(1, 0, 3, 2, 4).reshape(B, S, DIFF_HEADS, DIFF_V_DIM)
    o = rms_norm(o, out_g) * (1.0 - lambda_init)
    return o.reshape(B, S, DIFF_HEADS * DIFF_V_DIM)


def gla(q, k, v, r, a, w_gate2, b_gate, out_g):
    B, S = q.shape[0], q.shape[1]
    n_c = S // CHUNK
    pre = (a @ w_gate2 + b_gate).astype(jnp.float32)
    log_alpha = (jax.nn.log_sigmoid(pre) / GLA_TAU).reshape(B, S, GLA_HEADS, GLA_K_DIM)

    def chunked(t):
        return t.reshape(B, n_c, CHUNK, GLA_HEADS, t.shape[-1]).transpose(0, 3, 1, 2, 4).astype(jnp.float32)

    bcum = jnp.cumsum(chunked(log_alpha), axis=3)
    b_last = bcum[:, :, :, -1:, :]
    qf = chunked(q) * (GLA_K_DIM ** -0.5)
    kf = chunked(k)
    vf = chunked(v)
    q_in = qf * jnp.exp(bcum)
    k_in = kf * jnp.exp(-bcum)
    k_dec = kf * jnp.exp(b_last - bcum)
    tril = jnp.tril(jnp.ones((CHUNK, CHUNK), dtype=bool))
    att = jnp.where(tril, jnp.einsum('bhcid,bhcjd->bhcij', q_in, k_in), 0.0)
    o_intra = jnp.einsum('bhcij,bhcje->bhcie', att, vf)
    u = jnp.einsum('bhcld,bhcle->bhcde', k_dec, vf)
    decay = jnp.exp(b_last[:, :, :, 0, :])

    def step(state, inp):
        u_c, d_c = inp
        return d_c[..., None] * state + u_c, state

    s0 = jnp.zeros((B, GLA_HEADS, GLA_K_DIM, GLA_V_DIM), jnp.float32)
    _, s_in = lax.scan(step, s0, (jnp.moveaxis(u, 2, 0), jnp.moveaxis(decay, 2, 0)))
    s_in = jnp.moveaxis(s_in, 0, 2)
    o_inter = jnp.einsum('bhcld,bhcde->bhcle', q_in, s_in)
    o = (o_intra + o_inter).transpose(0, 2, 3, 1, 4).reshape(B, S, GLA_HEADS, GLA_V_DIM)
    o = rms_norm(o, out_g).reshape(B, S, GLA_HEADS * GLA_V_DIM)
    return (o * jax.nn.silu(r.astype(jnp.float32))).astype(r.dtype)


def hier_moe(h, w_rg, b_rg, w_re, b_re, w_gate, w_up, w_down):
    B, S, D = h.shape
    t = h.reshape(B * S, D)
    lg = jnp.einsum('td,dg->tg', t, w_rg, preferred_element_type=jnp.float32) + b_rg.astype(jnp.float32)
    pg = jax.nn.softmax(lg, axis=-1)
    g_sel = jnp.argmax(pg, axis=-1)
    pg_sel = jnp.take_along_axis(pg, g_sel[:, None], axis=-1)[:, 0]
    le = jnp.einsum('td,gde->tge', t, w_re, preferred_element_type=jnp.float32) + b_re.astype(jnp.float32)
    le_sel = jnp.take_along_axis(le, g_sel[:, None, None], axis=1)[:, 0]
    pe = jax.nn.softmax(le_sel, axis=-1)
    top_v, top_i = lax.top_k(pe, TOP_K)
    top_v = top_v / jnp.sum(top_v, axis=-1, keepdims=True)
    ew = jnp.sum(jax.nn.one_hot(top_i, EXPERTS_PER_GROUP, dtype=jnp.float32) * top_v[..., None], axis=1)
    combine = (jax.nn.one_hot(g_sel, N_GROUPS, dtype=jnp.float32)[:, :, None]
               * ew[:, None, :] * pg_sel[:, None, None])
    out = jnp.zeros_like(t)
    for g in range(N_GROUPS):
        hg = jax.nn.silu(jnp.einsum('td,edf->tef', t, w_gate[g])) * jnp.einsum('td,edf->tef', t, w_up[g])
        hg = hg * combine[:, g, :, None].astype(hg.dtype)
        out = out + jnp.einsum('tef,efd->td', hg, w_down[g])
    return out.reshape(B, S, D)


def setup_inputs(seed: int = 0) -> dict:
    key = jax.random.key(seed)
    ks = jax.random.split(key, 24)
    nrm = lambda k, s: jax.random.normal(k, s, dtype=jnp.float32)
    gain = lambda k, s: 1.0 + 0.02 * nrm(k, s)
    E, G, F = EXPERTS_PER_GROUP, N_GROUPS, D_FF_EXPERT
    offsets = jax.random.randint(ks[1], (BATCH, 1), 0, 256) * CHUNK
    positions = (offsets + jnp.arange(SEQ)[None, :]).astype(jnp.int32)
    return {
        "x": nrm(ks[0], (BATCH, SEQ, D_MODEL)),
        "positions": positions,
        "norm1_g": gain(ks[2], (DEPTH, D_MODEL)),
        "w_in": nrm(ks[3], (DEPTH, D_MODEL, D_IN)) * D_MODEL ** -0.5,
        "q_norm_g": gain(ks[4], (DEPTH, DIFF_QK_DIM)),
        "k_norm_g": gain(ks[5], (DEPTH, DIFF_QK_DIM)),
        "lambda_q1": 0.1 * nrm(ks[6], (DEPTH, DIFF_QK_DIM)),
        "lambda_k1": 0.1 * nrm(ks[7], (DEPTH, DIFF_QK_DIM)),
        "lambda_q2": 0.1 * nrm(ks[8], (DEPTH, DIFF_QK_DIM)),
        "lambda_k2": 0.1 * nrm(ks[9], (DEPTH, DIFF_QK_DIM)),
        "diff_out_norm_g": gain(ks[10], (DEPTH, DIFF_V_DIM)),
        "gla_w_gate2": nrm(ks[11], (DEPTH, GLA_GATE_RANK, GLA_HEADS * GLA_K_DIM)) * GLA_GATE_RANK ** -0.5,
        "gla_b_gate": 0.1 * nrm(ks[12], (DEPTH, GLA_HEADS * GLA_K_DIM)),
        "gla_out_norm_g": gain(ks[13], (DEPTH, GLA_V_DIM)),
        "w_out": nrm(ks[14], (DEPTH, D_MIX, D_MODEL)) * D_MIX ** -0.5,
        "norm2_g": gain(ks[15], (DEPTH, D_MODEL)),
        "w_router_group": nrm(ks[16], (DEPTH, D_MODEL, G)) * D_MODEL ** -0.5,
        "b_router_group": 0.01 * nrm(ks[17], (DEPTH, G)),
        "w_router_expert": nrm(ks[18], (DEPTH, G, D_MODEL, E)) * D_MODEL ** -0.5,
        "b_router_expert": 0.01 * nrm(ks[19], (DEPTH, G, E)),
        "w_gate_expert": nrm(ks[20], (DEPTH, G, E, D_MODEL, F)) * D_MODEL ** -0.5,
        "w_up_expert": nrm(ks[21], (DEPTH, G, E, D_MODEL, F)) * D_MODEL ** -0.5,
        "w_down_expert": nrm(ks[22], (DEPTH, G, E, F, D_MODEL)) * F ** -0.5,
    }


def reference(x, positions, norm1_g, w_in, q_norm_g, k_norm_g, lambda_q1, lambda_k1,
              lambda_q2, lambda_k2, diff_out_norm_g, gla_w_gate2, gla_b_gate, gla_out_norm_g,
              w_out, norm2_g, w_router_group, b_router_group, w_router_expert, b_router_expert,
              w_gate_expert, w_up_expert, w_down_expert):
    B, S, _ = x.shape
    split_idx = [int(v) for v in np.cumsum(IN_SPLITS)[:-1]]
    for l in range(DEPTH):
        lambda_init = 0.8 - 0.6 * math.exp(-0.3 * l)
        h = rms_norm(x, norm1_g[l])
        proj = h @ w_in[l]
        dq, dk, dv, gq, gk, gv, gr, ga = jnp.split(proj, split_idx, axis=-1)
        a_out = diff_attention(
            dq.reshape(B, S, DIFF_HEADS, 2, DIFF_QK_DIM),
            dk.reshape(B, S, DIFF_HEADS, 2, DIFF_QK_DIM),
            dv.reshape(B, S, DIFF_HEADS, DIFF_V_DIM),
            positions, q_norm_g[l], k_norm_g[l], lambda_q1[l], lambda_k1[l],
            lambda_q2[l], lambda_k2[l], diff_out_norm_g[l], lambda_init)
        g_out = gla(
            gq.reshape(B, S, GLA_HEADS, GLA_K_DIM),
            gk.reshape(B, S, GLA_HEADS, GLA_K_DIM),
            gv.reshape(B, S, GLA_HEADS, GLA_V_DIM),
            gr, ga, gla_w_gate2[l], gla_b_gate[l], gla_out_norm_g[l])
        mixed = jnp.concatenate([a_out, g_out.astype(a_out.dtype)], axis=-1) @ w_out[l]
        x = x + mixed
        x = x + hier_moe(rms_norm(x, norm2_g[l]), w_router_group[l], b_router_group[l],
                         w_router_expert[l], b_router_expert[l], w_gate_expert[l],
                         w_up_expert[l], w_down_expert[l])
    return x
```

```python
import numpy as np
import concourse.bass as bass
import concourse.mybir as mybir
from concourse.bass_utils import run_bass_kernel_spmd

F32 = mybir.dt.float32
BF16 = mybir.dt.bfloat16
U8 = mybir.dt.uint8
I32 = mybir.dt.int32
AF = mybir.ActivationFunctionType
ALU = mybir.AluOpType
AX = mybir.AxisListType


STRICT_WAR = True


class Prog:
    ENGS = ("pe", "act", "dve", "pool", "sp")

    def __init__(self, nc):
        self.nc = nc
        self.ops = []

    def add(self, eng, fn, reads=(), writes=(), dma=None, nophase=False):
        reads = tuple(reads)
        if not nophase:
            reads = reads + ("PHASE",)
        self.ops.append(dict(eng=eng, fn=fn, reads=reads, writes=tuple(writes),
                             dma=dma, deps=set(), sig=False))

    def capture(self, fn):
        old_ops = self.ops
        self.ops = []
        ret = fn()
        got = self.ops
        self.ops = old_ops
        return got, ret

    @staticmethod
    def merge(a, b):
        out, ia, ib = [], 0, 0
        while ia < len(a) or ib < len(b):
            if ib >= len(b) or (ia < len(a) and ia * len(b) <= ib * len(a)):
                out.append(a[ia]); ia += 1
            else:
                out.append(b[ib]); ib += 1
        return out

    def barrier(self, scratch):
        self.add("dve", lambda e: e.memset(scratch, 0.0), reads=(), writes=("PHASE", "bar_scratch"), nophase=True)

    def _analyse(self):
        last_w = {}
        readers = {}
        for i, op in enumerate(self.ops):
            deps = set()
            for r in op["reads"]:
                if r in last_w:
                    deps.add((last_w[r], "raw"))
            for w in op["writes"]:
                if w in last_w:
                    deps.add((last_w[w], "waw"))
                for rd in readers.get(w, ()):
                    deps.add((rd, "war"))
            real = set()
            for (d, kind) in deps:
                if d == i:
                    continue
                dop = self.ops[d]
                if dop["dma"] is None and dop["eng"] == op["eng"] and op["dma"] is None:
                    if op["eng"] == "pe" or (kind == "war" and not STRICT_WAR):
                        continue
                real.add(d)
            op["deps"] = real
            for d in real:
                self.ops[d]["sig"] = True
            for r in op["reads"]:
                readers.setdefault(r, []).append(i)
            for w in op["writes"]:
                last_w[w] = i
                readers[w] = []

    def emit(self, sems, dma_sems):
        self._analyse()
        nc = self.nc
        cnt = {e: 0 for e in self.ENGS}
        dcnt = {}
        for op in self.ops:
            if op["dma"] is not None:
                k = op["dma"]
                dcnt[k] = dcnt.get(k, 0) + 16
                op["tok"] = (dma_sems[k], dcnt[k])
            elif op["sig"]:
                cnt[op["eng"]] += 1
                op["tok"] = (sems[op["eng"]], cnt[op["eng"]])
        by_eng = {e: [op for op in self.ops if op["eng"] == e] for e in self.ENGS}
        ops = self.ops

        def run(eng_handle, lst):
            have = {}
            for op in lst:
                need = {}
                for d in op["deps"]:
                    s, v = ops[d]["tok"]
                    key = id(s)
                    if need.get(key, (None, 0))[1] < v:
                        need[key] = (s, v)
                for key, (s, v) in need.items():
                    if have.get(key, 0) < v:
                        eng_handle.wait_ge(s, v)
                        have[key] = v
                ins = op["fn"](eng_handle)
                if op["dma"] is not None:
                    ins.then_inc(op["tok"][0], 16)
                elif op["sig"]:
                    ins.then_inc(op["tok"][0], 1)

        with nc.Block() as block:
            @block.tensor
            def _(e):
                run(e, by_eng["pe"])

            @block.scalar
            def _(e):
                run(e, by_eng["act"])

            @block.vector
            def _(e):
                run(e, by_eng["dve"])

            @block.gpsimd
            def _(e):
                run(e, by_eng["pool"])

            @block.sync
            def _(e):
                run(e, by_eng["sp"])


class Region:
    def __init__(self, big, lo, hi):
        self.big, self.lo, self.hi, self.cur = big, lo, hi, lo

    def reset(self):
        self.cur = self.lo

    def alloc(self, nbytes, dtype, shape=None):
        a = (self.cur + 31) // 32 * 32
        assert a + nbytes <= self.hi, ("region overflow", a, nbytes, self.hi)
        self.cur = a + nbytes
        ap = self.big[:, a:a + nbytes]
        if dtype is not U8:
            ap = ap.bitcast(dtype)
        if shape is not None and len(shape) == 2:
            ap = ap.rearrange("p (a b) -> p a b", b=shape[1])
        elif shape is not None and len(shape) == 3:
            ap = ap.rearrange("p (a b c) -> p a b c", b=shape[1], c=shape[2])
        return ap


DM = 2048
NTO = 8
NTA = 16
KC = 16
C_DQ, C_DK, C_DV, C_GQ, C_GK, C_GV, C_GR, C_GA = 0, 1024, 2048, 3072, 3584, 4096, 5120, 6144
D_IN = 6160
NE = 32
FF = 512
CAP = 256
EPS = 1e-6
LAMBDA_INIT = 0.2
TWO_PI = 2.0 * np.pi
CW1 = 6.28125
CW2 = TWO_PI - CW1

CA_ID, CA_TRI, CA_TRS, CA_ONE, CA_FLG, CA_DM = 0, 128, 256, 384, 512, 640
NCA = 640 + 4 * 512
CB_GQ, CB_GK, CB_LAM, CB_GOUT, CB_FLAG, CB_BG, CB_GOG, CB_RB, CB_INVF, CB_PB = 0, 64, 128, 384, 385, 392, 904, 1160, 1196, 1204
NCB = 1240


def build_program(stage="full"):
    nc = bass.Bass("TRN2", target_bir_lowering=False)

    def din(name, shape, dtype=F32):
        return nc.dram_tensor(name, shape, dtype, kind="ExternalInput").ap()

    x_own = din("x_own", [1024, DM])
    x_oth = din("x_oth", [1024, DM])
    pos_d = din("pos", [128, NTA], I32)
    cstA_d = din("cstA", [128, NCA])
    cstB_d = din("cstB", [128, NCB])
    g1_d = din("g1rep", [128, DM])
    g2_d = din("g2rep", [128, DM])
    wg2_d = din("wg2", [16, 512])
    w_in = din("w_in", [DM, D_IN])
    w_out = din("w_out", [DM, DM])
    wr_d = din("wr", [DM, 36])
    w_gate = din("w_gate", [NE, DM, FF])
    w_up = din("w_up", [NE, DM, FF])
    w_down = din("w_down", [NE, FF, DM])
    out_d = nc.dram_tensor("out", [1024, DM], F32, kind="ExternalOutput").ap()
    if stage != "full":
        dbg_d = nc.dram_tensor("dbg", [128, 8192], F32, kind="ExternalOutput").ap()
        dbgb_d = nc.dram_tensor("dbgb", [128, 16384], BF16, kind="ExternalOutput").ap()
    else:
        dbg_d = dbgb_d = None
    xg_d = nc.dram_tensor("xg_scr", [NE * CAP, DM], BF16, kind="Internal").ap()
    yb_d = nc.dram_tensor("y_scr", [NE * CAP, DM], BF16, kind="Internal").ap()
    x1_d = nc.dram_tensor("x1_scr", [1024, DM], F32, kind="Internal").ap()
    NCONV = 15
    wcb_d = nc.dram_tensor("wcb_scr", [NCONV * 3, 128, 8192], BF16, kind="Internal").ap()

    w_in_v = w_in.rearrange("(kc p) c -> p kc c", p=128)
    w_out_v = w_out.rearrange("(kc p) c -> p kc c", p=128)
    wr_v = wr_d.rearrange("(kc p) c -> p kc c", p=128)

    dma_keys = []

    def K(k):
        if k not in dma_keys:
            dma_keys.append(k)
        return k

    import contextlib
    with contextlib.ExitStack() as es:
        big = es.enter_context(nc.sbuf_tensor("big", [128, 207 * 1024], U8))
        ps = [es.enter_context(nc.psum_tensor("ps%d" % i, [128, 512], F32)) for i in range(8)]
        psb = [p[:, :].bitcast(BF16) for p in ps]
        P = Prog(nc)
        RA = Region(big, 0, 64 * 1024)
        RB = Region(big, 64 * 1024, 96 * 1024)
        RC = Region(big, 96 * 1024, 128 * 1024)
        RD = Region(big, 128 * 1024, 190 * 1024)
        RE = Region(big, 190 * 1024, 207 * 1024)

        PSR = lambda b: ("ps", b)

        ident_bf = RE.alloc(256, BF16)
        ones_bf = RE.alloc(256, BF16)
        flag_bf = RE.alloc(256, BF16)
        trs_bf = RE.alloc(256, BF16)
        dmask_bf = RE.alloc(4 * 1024, BF16, [4, 512])
        tri_f = RE.alloc(512, F32)
        ones_f = RE.alloc(512, F32)
        cB = RE.alloc(NCB * 4, F32)
        wg2_f = RE.alloc(2048, F32)
        wr_bf = RE.alloc(KC * 36 * 2, BF16, [KC, 36])
        cos_t = RE.alloc(NTA * 8 * 4, F32, [NTA, 8])
        sin_t = RE.alloc(NTA * 8 * 4, F32, [NTA, 8])
        neglam = RE.alloc(4, F32)
        gout_s = RE.alloc(4, F32)
        gq_s = RE.alloc(256, F32)
        sm = RE.alloc(128 * 4, F32)
        flagcol = cB[:, CB_FLAG:CB_FLAG + 1]
        bar = RE.alloc(32, F32)

        hT = RA.alloc(KC * 2048 * 2, BF16, [KC, 2048])
        wbuf = [RB.alloc(KC * 512 * 2, BF16, [KC, 512]) for _ in range(2)]
        mixT = RC.alloc(KC * 1024 * 2, BF16, [KC, 1024])

        RD.reset()
        cA = RD.alloc(NCA * 4, F32)
        pos_i = RD.alloc(NTA * 4, I32)
        pos_f = RD.alloc(NTA * 4, F32)
        ang = RD.alloc(NTA * 8 * 4, F32, [NTA, 8])
        angc = RD.alloc(NTA * 8 * 4, F32, [NTA, 8])
        kf = RD.alloc(NTA * 8 * 4, F32, [NTA, 8])
        ki = RD.alloc(NTA * 8 * 4, I32, [NTA, 8])
        mk = RD.alloc(NTA * 8 * 4, F32, [NTA, 8])
        g1 = RD.alloc(DM * 4, F32)
        xt = [RD.alloc(DM * 4, F32) for _ in range(3)]
        hb = [RD.alloc(DM * 2, BF16) for _ in range(2)]
        sqj = RD.alloc(DM * 2, BF16)
        ss1 = RD.alloc(NTA * 4, F32)
        rs1 = RD.alloc(NTA * 4, F32)

        P.add("sp", lambda e: e.dma_start(out=cA, in_=cstA_d[:, :]), writes=["cA"], dma=K("cA"))
        P.add("sp", lambda e: e.dma_start(out=cB, in_=cstB_d[:, :]), writes=["cB"], dma=K("cB"))
        P.add("sp", lambda e: e.dma_start(out=pos_i, in_=pos_d[:, :]), writes=["pos_i"], dma=K("pos"))
        P.add("sp", lambda e: e.dma_start(out=wg2_f[0:16, :], in_=wg2_d[:, :]), writes=["wg2"], dma=K("wg2"))
        P.add("sp", lambda e: e.dma_start(out=g1, in_=g1_d[:, :]), writes=["g1"], dma=K("g1"))
        P.add("pool", lambda e: e.dma_start(out=wr_bf, in_=wr_v), writes=["wr"], dma=K("wr"))
        for (dst, c0, n, nm) in ((ident_bf, CA_ID, 128, "ident"), (ones_bf, CA_ONE, 128, "ones"), (flag_bf, CA_FLG, 128, "flagm"),
                                 (trs_bf, CA_TRS, 128, "trs"), (dmask_bf.rearrange("p a b -> p (a b)"), CA_DM, 2048, "dmask"),
                                 (tri_f, CA_TRI, 128, "tri_f"), (ones_f, CA_ONE, 128, "ones_f")):
            P.add("dve", (lambda dst, c0, n: lambda e: e.tensor_copy(out=dst, in_=cA[:, c0:c0 + n]))(dst, c0, n),
                  reads=["cA"], writes=[nm])

        P.add("dve", lambda e: e.tensor_copy(out=pos_f, in_=pos_i), reads=["pos_i"], writes=["pos_f"])
        P.add("dve", lambda e: e.tensor_tensor(out=ang, in0=pos_f.unsqueeze(2).to_broadcast([128, NTA, 8]),
                                               in1=cB[:, CB_INVF:CB_INVF + 8].unsqueeze(1).to_broadcast([128, NTA, 8]), op=ALU.mult),
              reads=["pos_f", "cB"], writes=["ang"])
        P.add("dve", lambda e: e.tensor_scalar_add(out=angc, in0=ang, scalar1=float(np.pi / 2)), reads=["ang"], writes=["angc"])

        def range_reduce(a, nm):
            P.add("dve", lambda e: e.tensor_scalar_mul(out=ki, in0=a, scalar1=float(1.0 / TWO_PI)), reads=[nm], writes=["ki"])
            P.add("dve", lambda e: e.tensor_copy(out=kf, in_=ki), reads=["ki"], writes=["kf"])
            P.add("dve", lambda e: e.scalar_tensor_tensor(out=a, in0=kf, scalar=-CW1, in1=a, op0=ALU.mult, op1=ALU.add),
                  reads=["kf", nm], writes=[nm])
            P.add("dve", lambda e: e.scalar_tensor_tensor(out=a, in0=kf, scalar=-CW2, in1=a, op0=ALU.mult, op1=ALU.add),
                  reads=["kf", nm], writes=[nm])
            P.add("dve", lambda e: e.tensor_single_scalar(out=mk, in_=a, scalar=float(np.pi), op=ALU.is_gt), reads=[nm], writes=["mk"])
            P.add("dve", lambda e: e.scalar_tensor_tensor(out=a, in0=mk, scalar=-TWO_PI, in1=a, op0=ALU.mult, op1=ALU.add),
                  reads=["mk", nm], writes=[nm])
            P.add("dve", lambda e: e.tensor_single_scalar(out=mk, in_=a, scalar=float(-np.pi), op=ALU.is_lt), reads=[nm], writes=["mk"])
            P.add("dve", lambda e: e.scalar_tensor_tensor(out=a, in0=mk, scalar=TWO_PI, in1=a, op0=ALU.mult, op1=ALU.add),
                  reads=["mk", nm], writes=[nm])
            P.add("dve", lambda e: e.tensor_scalar(out=a, in0=a, scalar1=3.1415925, scalar2=-3.1415925, op0=ALU.min, op1=ALU.max),
                  reads=[nm], writes=[nm])

        range_reduce(ang, "ang")
        range_reduce(angc, "angc")
        P.add("act", lambda e: e.activation(out=sin_t, in_=ang, func=AF.Sin), reads=["ang"], writes=["sin"])
        P.add("act", lambda e: e.activation(out=cos_t, in_=angc, func=AF.Sin), reads=["angc"], writes=["cos"])

        lv = cB[:, CB_LAM:CB_LAM + 256]
        P.add("dve", lambda e: e.tensor_tensor(out=sm[:, 0:64], in0=lv[:, 0:64], in1=lv[:, 64:128], op=ALU.mult), reads=["cB"], writes=["sm"])
        P.add("dve", lambda e: e.tensor_reduce(out=sm[:, 64:65], in_=sm[:, 0:64], axis=AX.X, op=ALU.add), reads=["sm"], writes=["sm1"])
        P.add("dve", lambda e: e.tensor_tensor(out=sm[:, 0:64], in0=lv[:, 128:192], in1=lv[:, 192:256], op=ALU.mult),
              reads=["cB", "sm1"], writes=["sm"])
        P.add("dve", lambda e: e.tensor_reduce(out=sm[:, 65:66], in_=sm[:, 0:64], axis=AX.X, op=ALU.add), reads=["sm"], writes=["sm2"])
        P.add("act", lambda e: e.activation(out=sm[:, 66:68], in_=sm[:, 64:66], func=AF.Exp), reads=["sm1", "sm2"], writes=["sm3"])
        P.add("dve", lambda e: e.tensor_tensor(out=neglam, in0=sm[:, 67:68], in1=sm[:, 66:67], op=ALU.subtract), reads=["sm3"], writes=["neglam"])
        P.add("dve", lambda e: e.tensor_scalar_add(out=neglam, in0=neglam, scalar1=-LAMBDA_INIT), reads=["neglam"], writes=["neglam"])
        P.add("dve", lambda e: e.tensor_scalar_mul(out=gout_s, in0=cB[:, CB_GOUT:CB_GOUT + 1], scalar1=1.0 - LAMBDA_INIT),
              reads=["cB"], writes=["gout_s"])
        P.add("dve", lambda e: e.tensor_scalar_mul(out=gq_s, in0=cB[:, CB_GQ:CB_GQ + 64], scalar1=0.125), reads=["cB"], writes=["gq_s"])
        gk_g = cB[:, CB_GK:CB_GK + 64]

        def x_tile_src(j):
            return (x_own if j < NTO else x_oth)[(j % NTO) * 128:(j % NTO + 1) * 128, :]

        for j in range(NTA):
            b = j % 3
            P.add("sp", (lambda j, b: lambda e: e.dma_start(out=xt[b], in_=x_tile_src(j)))(j, b), writes=[("xt", b)], dma=K(("xt", b)))
            P.add("act", (lambda j, b: lambda e: e.activation(out=sqj, in_=xt[b], func=AF.Square, accum_out=ss1[:, j:j + 1]))(j, b),
                  reads=[("xt", b)], writes=["sqj", ("ss1", j)])
            P.add("act", (lambda j: lambda e: e.activation(out=rs1[:, j:j + 1], in_=ss1[:, j:j + 1], func=AF.Ln, scale=1.0 / DM, bias=EPS))(j),
                  reads=[("ss1", j)], writes=[("rs1", j)])
            P.add("act", (lambda j: lambda e: e.activation(out=rs1[:, j:j + 1], in_=rs1[:, j:j + 1], func=AF.Exp, scale=-0.5))(j),
                  reads=[("rs1", j)], writes=[("rs1", j)])
            P.add("dve", (lambda j, b: lambda e: e.scalar_tensor_tensor(out=hb[j % 2], in0=xt[b], scalar=rs1[:, j:j + 1], in1=g1,
                                                                         op0=ALU.mult, op1=ALU.mult))(j, b),
                  reads=[("xt", b), ("rs1", j), "g1"], writes=[("hb", j % 2)])
            for half in range(2):
                bank = 2 + half
                for i in range(8):
                    kc = half * 8 + i
                    P.add("pe", (lambda j, kc, i, bank: lambda e: e.transpose(out=psb[bank][:, i * 128:(i + 1) * 128],
                                                                              in_=hb[j % 2][:, kc * 128:(kc + 1) * 128], identity=ident_bf))(j, kc, i, bank),
                          reads=[("hb", j % 2), "ident"], writes=[PSR(bank)])
                eng = "act" if half == 0 else "dve"
                if eng == "act":
                    P.add("act", (lambda j, half, bank: lambda e: e.activation(out=hT[:, half * 8:half * 8 + 8, j * 128:(j + 1) * 128],
                                                                               in_=psb[bank].rearrange("p (a b) -> p a b", b=128), func=AF.Copy))(j, half, bank),
                          reads=[], writes=[PSR(bank), ("hT", j)])
                else:
                    P.add("dve", (lambda j, half, bank: lambda e: e.tensor_copy(out=hT[:, half * 8:half * 8 + 8, j * 128:(j + 1) * 128],
                                                                                in_=psb[bank].rearrange("p (a b) -> p a b", b=128)))(j, half, bank),
                          reads=[], writes=[PSR(bank), ("hT", j)])

        final_reads = []
        conv_next = [0]

        def emit_conv(k):
            for _ in range(k):
                m = conv_next[0]
                if m >= NCONV * 3:
                    return
                conv_next[0] += 1
                e_ = NE - NCONV + m // 3
                kind = m % 3
                if kind == 0:
                    src = w_gate[e_].rearrange("(kc p) f -> p kc f", p=128)
                    dst = wcb_d[m].rearrange("p (a b) -> p a b", b=FF)
                elif kind == 1:
                    src = w_up[e_].rearrange("(kc p) f -> p kc f", p=128)
                    dst = wcb_d[m].rearrange("p (a b) -> p a b", b=FF)
                else:
                    src = w_down[e_].rearrange("(fc p) d -> p fc d", p=128)
                    dst = wcb_d[m].rearrange("p (a b) -> p a b", b=DM)
                P.add("pool", (lambda src, dst: lambda e: e.dma_start(out=dst, in_=src))(src, dst), writes=[("conv", m)], dma=K("conv"), nophase=True)

        ctx = dict(emit_conv=emit_conv, NCONV=NCONV, wcb_d=wcb_d, nc=nc, P=P, K=K, ps=ps, psb=psb, PSR=PSR, hT=hT, wbuf=wbuf, mixT=mixT, w_in_v=w_in_v,
                   RA=RA, RB=RB, RC=RC, RD=RD, RE=RE, ident_bf=ident_bf, ones_bf=ones_bf, flag_bf=flag_bf, trs_bf=trs_bf,
                   dmask_bf=dmask_bf, tri_f=tri_f, ones_f=ones_f, cB=cB, wg2_f=wg2_f, wr_bf=wr_bf, cos_t=cos_t, sin_t=sin_t,
                   bar=bar, neglam=neglam, gout_s=gout_s, gq_s=gq_s, gk_g=gk_g, flagcol=flagcol, sm=sm,
                   dbg_d=dbg_d, dbgb_d=dbgb_d, out_d=out_d, x_own=x_own, xg_d=xg_d, yb_d=yb_d, x1_d=x1_d, g2_d=g2_d,
                   w_out_v=w_out_v, w_gate=w_gate, w_up=w_up, w_down=w_down, stage=stage, final_reads=final_reads)

        if stage == "p1":
            RD.reset()
            d1 = RD.alloc(8192 * 4, F32)
            P.add("dve", lambda e: e.tensor_copy(out=d1[:, 0:4096].rearrange("p (a b) -> p a b", b=2048), in_=hT[:, 0:2, :]),
                  reads=[("hT", j) for j in range(NTA)], writes=["d1"])
            P.add("dve", lambda e: e.tensor_copy(out=d1[:, 4096:4096 + 128], in_=cos_t.rearrange("p a b -> p (a b)")), reads=["cos"], writes=["d1b"])
            P.add("dve", lambda e: e.tensor_copy(out=d1[:, 4224:4224 + 128], in_=sin_t.rearrange("p a b -> p (a b)")), reads=["sin"], writes=["d1c"])
            P.add("dve", lambda e: e.tensor_copy(out=d1[:, 4352:4353], in_=neglam), reads=["neglam"], writes=["d1d"])
            P.add("sp", lambda e: e.dma_start(out=dbg_d[:, :], in_=d1), reads=["d1", "d1b", "d1c", "d1d"], writes=["dbg"], dma=K("dbg"))
        else:
            mixer_stage(ctx)

        P.add("sp", lambda e: e.nop(), reads=["dbg"] + final_reads)
        sem_e = {k: es.enter_context(nc.semaphore("s_" + k)) for k in Prog.ENGS}
        sem_d = {k: es.enter_context(nc.semaphore("d%d" % i)) for i, k in enumerate(dma_keys)}
        P.emit(sem_e, sem_d)
    return nc


def _consts(flag):
    t = np.arange(128)
    cA = np.zeros((128, NCA), np.float32)
    cA[:, CA_ID:CA_ID + 128] = np.eye(128, dtype=np.float32)
    cA[:, CA_TRI:CA_TRI + 128] = (t[:, None] <= t[None, :])
    cA[:, CA_TRS:CA_TRS + 128] = (t[:, None] < t[None, :])
    cA[:, CA_ONE:CA_ONE + 128] = 1.0
    cA[:, CA_FLG:CA_FLG + 128] = flag
    for r in range(4):
        for c in range(4):
            blk = cA[:, CA_DM + r * 512 + c * 128: CA_DM + r * 512 + (c + 1) * 128]
            if c > r:
                blk[:] = 1.0
            elif c == r:
                blk[:] = (t[:, None] // 64 <= t[None, :] // 64)
    return cA


def prep_inputs(inputs, cores=range(8)):
    f = lambda k: np.ascontiguousarray(np.asarray(inputs[k]))
    x = f("x")
    pos = f("positions")
    rep = lambda v: np.ascontiguousarray(np.broadcast_to(np.asarray(v, np.float32).reshape(1, -1), (128, np.asarray(v).size)))
    cB0 = np.zeros((128, NCB), np.float32)
    cB0[:, CB_GQ:CB_GQ + 64] = rep(f("q_norm_g")[0])
    cB0[:, CB_GK:CB_GK + 64] = rep(f("k_norm_g")[0])
    cB0[:, CB_LAM:CB_LAM + 256] = rep(np.concatenate([f("lambda_q1")[0], f("lambda_k1")[0], f("lambda_q2")[0], f("lambda_k2")[0]]))
    cB0[:, CB_GOUT] = f("diff_out_norm_g")[0]
    cB0[:, CB_BG:CB_BG + 512] = rep(f("gla_b_gate")[0])
    cB0[:, CB_GOG:CB_GOG + 256] = rep(f("gla_out_norm_g")[0])
    cB0[:, CB_RB:CB_RB + 36] = rep(np.concatenate([f("b_router_group")[0], f("b_router_expert")[0].reshape(-1)]))
    cB0[:, CB_INVF:CB_INVF + 8] = rep(np.float32(500000.0) ** (-np.arange(0, 16, 2, dtype=np.float32) / np.float32(16)))
    cB0[:, CB_PB:CB_PB + 32] = rep(np.arange(32, dtype=np.float32) * CAP)
    wr = np.ascontiguousarray(np.concatenate([f("w_router_group")[0], f("w_router_expert")[0].transpose(1, 0, 2).reshape(DM, 32)], axis=1))
    shared = dict(
        g1rep=rep(f("norm1_g")[0]), g2rep=rep(f("norm2_g")[0]), wg2=f("gla_w_gate2")[0],
        w_in=f("w_in")[0], w_out=f("w_out")[0], wr=wr,
        w_gate=f("w_gate_expert")[0].reshape(NE, DM, FF), w_up=f("w_up_expert")[0].reshape(NE, DM, FF),
        w_down=f("w_down_expert")[0].reshape(NE, FF, DM))
    maps = []
    for c in cores:
        b, half = c // 2, c % 2
        cB = cB0.copy()
        cB[:, CB_FLAG] = float(half)
        po = pos[b, half * 1024:(half + 1) * 1024].reshape(8, 128)
        pt = pos[b, 0:1024].reshape(8, 128)
        m = dict(shared)
        m.update(x_own=np.ascontiguousarray(x[b, half * 1024:(half + 1) * 1024]), x_oth=np.ascontiguousarray(x[b, 0:1024]),
                 pos=np.ascontiguousarray(np.concatenate([po, pt], 0).T.astype(np.int32)),
                 cstA=_consts(float(half)), cstB=cB)
        maps.append(m)
    return maps


_NC_CACHE = {}


def kernel(**inputs):
    if "full" not in _NC_CACHE:
        _NC_CACHE["full"] = build_program("full")
    nc = _NC_CACHE["full"]
    maps = prep_inputs(inputs)
    res = run_bass_kernel_spmd(nc, maps, core_ids=list(range(8)))
    out = np.zeros((4, 2048, DM), np.float32)
    for c in range(8):
        b, half = c // 2, c % 2
        out[b, half * 1024:(half + 1) * 1024] = res.results[c]["out"]
    return out


def mixer_stage(ctx):
    g = dict(ctx)
    nc, P, K, ps, psb, PSR = (g[k] for k in ("nc", "P", "K", "ps", "psb", "PSR"))
    hT, wbuf, mixT, w_in_v, RD, cB = (g[k] for k in ("hT", "wbuf", "mixT", "w_in_v", "RD", "cB"))
    ident_bf, ones_bf, flag_bf, dmask_bf = (g[k] for k in ("ident_bf", "ones_bf", "flag_bf", "dmask_bf"))
    cos_t, sin_t, neglam, gout_s, gq_s, gk_g, flagcol = (g[k] for k in ("cos_t", "sin_t", "neglam", "gout_s", "gq_s", "gk_g", "flagcol"))
    stage = g["stage"]
    OWN = list(range(NTO))
    ALL = list(range(NTA))
    wslot = [0]
    mmbank = [0]
    mmbanks = [[0, 1]]

    emit_conv = g["emit_conv"]

    def load_w(c0, n):
        s = wslot[0]
        wslot[0] ^= 1
        P.add("pool", lambda e: e.dma_start(out=wbuf[s][:, :, 0:n], in_=w_in_v[:, :, c0:c0 + n]), writes=[("wb", s)], dma=K(("wb", s)), nophase=True)
        emit_conv(1)
        return s

    def proj_tile(s, n, j):
        bl = mmbanks[0]
        b = bl[mmbank[0] % len(bl)]
        mmbank[0] += 1
        for kc in range(KC):
            P.add("pe", (lambda kc: lambda e: e.matmul(ps[b][:, 0:n], lhsT=hT[:, kc, j * 128:(j + 1) * 128], rhs=wbuf[s][:, kc, 0:n],
                                                       start=(kc == 0), stop=(kc == KC - 1)))(kc),
                  reads=[("hT", j), ("wb", s)], writes=[PSR(b)])
        return b

    bar = g["bar"]
    P.barrier(bar)
    RD.reset()
    qT = RD.alloc(4 * 1024 * 2, BF16, [4, 1024])
    kT = RD.alloc(4 * 2048 * 2, BF16, [4, 2048])
    vv = RD.alloc(NTA * 512 * 2, BF16, [NTA, 512])
    shared_lo = RD.cur
    CT = []
    NCT = 3
    for _ in range(NCT):
        CT.append(dict(sqt=RD.alloc(2048, F32), qn=RD.alloc(2048, F32, [8, 64]), qb=RD.alloc(1024, BF16, [8, 64]), ssq=RD.alloc(32, F32),
                       rsq=RD.alloc(32, F32), rx=RD.alloc(8 * 16 * 4, F32, [8, 16]), rt=[RD.alloc(8 * 8 * 4, F32, [8, 8]) for _ in range(4)]))
    RD.cur = shared_lo
    Pm = [[RD.alloc(1024, BF16) for _ in range(2)] for _ in range(2)]
    rl = [RD.alloc(2048, F32) for _ in range(2)]
    tt = [RD.alloc(2048, F32) for _ in range(2)]
    ot = RD.alloc(2048, F32)
    osq = RD.alloc(1024, BF16)
    rstd_o = rl[0]
    ccnt = [0]

    xg_d = g["xg_d"]
    zsrc = vv[:, 0:4, :].rearrange("p a b -> p (a b)")
    P.add("dve", lambda e: e.memset(zsrc, 0.0), reads=[], writes=[("v", jj) for jj in range(4)])
    for u_ in range(NE * CAP // 128):
        P.add("sp", (lambda u_: lambda e: e.dma_start(out=xg_d[u_ * 128:(u_ + 1) * 128, :], in_=zsrc))(u_), reads=[("v", jj) for jj in range(4)],
              writes=[("xgd", u_)], dma=K("xgz"), nophase=True)

    def qk_consumer(j, b, is_q, dstT):
        gv = gq_s if is_q else gk_g
        nm = "q" if is_q else "k"
        cp = ccnt[0] % NCT
        ccnt[0] += 1
        c = CT[cp]
        sqt, qn, qb, ssq, rsq, rx, rt = c["sqt"], c["qn"], c["qb"], c["ssq"], c["rsq"], c["rx"], c["rt"]
        R = lambda x: (x, cp)
        tb = 2 + cp % 2
        psv = ps[b][:, 0:512]

        def stA():
            P.add("act", lambda e: e.activation(out=sqt, in_=psv, func=AF.Square), reads=[], writes=[PSR(b), R("sqt")])
            P.add("dve", lambda e: e.tensor_reduce(out=ssq, in_=sqt.rearrange("p (a b) -> p a b", b=64), axis=AX.X, op=ALU.add),
                  reads=[R("sqt")], writes=[R("ssq")])
            P.add("act", lambda e: e.activation(out=rsq, in_=ssq, func=AF.Ln, scale=1.0 / 64, bias=EPS), reads=[R("ssq")], writes=[R("rsq")])
            P.add("act", lambda e: e.activation(out=rsq, in_=rsq, func=AF.Exp, scale=-0.5), reads=[R("rsq")], writes=[R("rsq")])
            P.add("dve", lambda e: e.tensor_tensor(out=qn, in0=psv.rearrange("p (a b) -> p a b", b=64),
                                                   in1=rsq.unsqueeze(2).to_broadcast([128, 8, 64]), op=ALU.mult),
                  reads=[R("rsq")], writes=[PSR(b), R("qn")])

        def stB():
            P.add("pool", lambda e: e.tensor_tensor(out=qb[:, :, 16:64], in0=qn[:, :, 16:64], in1=gv[:, 16:64].unsqueeze(1).to_broadcast([128, 8, 48]), op=ALU.mult),
                  reads=[R("qn"), "gq_s", "cB"], writes=[R("qbhi")])
            P.add("dve", lambda e: e.tensor_tensor(out=rx, in0=qn[:, :, 0:16], in1=gv[:, 0:16].unsqueeze(1).to_broadcast([128, 8, 16]), op=ALU.mult),
                  reads=[R("qn"), "gq_s", "cB"], writes=[R("rx")])
            cj = cos_t[:, j, :].unsqueeze(1).to_broadcast([128, 8, 8])
            sj = sin_t[:, j, :].unsqueeze(1).to_broadcast([128, 8, 8])
            x1, x2 = rx[:, :, 0:8], rx[:, :, 8:16]
            P.add("dve", lambda e: e.tensor_tensor(out=rt[0], in0=x1, in1=cj, op=ALU.mult), reads=[R("rx"), "cos"], writes=[R("rt0")])
            P.add("dve", lambda e: e.tensor_tensor(out=rt[1], in0=x2, in1=sj, op=ALU.mult), reads=[R("rx"), "sin"], writes=[R("rt1")])
            P.add("dve", lambda e: e.tensor_tensor(out=rt[2], in0=x2, in1=cj, op=ALU.mult), reads=[R("rx"), "cos"], writes=[R("rt2")])
            P.add("dve", lambda e: e.tensor_tensor(out=rt[3], in0=x1, in1=sj, op=ALU.mult), reads=[R("rx"), "sin"], writes=[R("rt3")])
            P.add("dve", lambda e: e.tensor_tensor(out=qb[:, :, 0:8], in0=rt[0], in1=rt[1], op=ALU.subtract), reads=[R("rt0"), R("rt1")], writes=[R("qblo1")])
            P.add("dve", lambda e: e.tensor_tensor(out=qb[:, :, 8:16], in0=rt[2], in1=rt[3], op=ALU.add), reads=[R("rt2"), R("rt3")], writes=[R("qblo2")])

        def stC():
            for h in range(4):
                P.add("pe", (lambda h: lambda e: e.transpose(out=psb[tb][:, h * 128:(h + 1) * 128],
                                                             in_=qb[:, 2 * h:2 * h + 2, :].rearrange("p a b -> p (a b)"), identity=ident_bf))(h),
                      reads=[R("qbhi"), R("qblo1"), R("qblo2"), "ident"], writes=[PSR(tb)])
            P.add("act", lambda e: e.activation(out=dstT[:, :, j * 128:(j + 1) * 128], in_=psb[tb][:, 0:512].rearrange("p (a b) -> p a b", b=128),
                                                func=AF.Copy), reads=[], writes=[PSR(tb), (nm + "T", j)])
        return [stA, stB, stC]

    def v_consumer(j, b):
        psv = ps[b][:, 0:512]
        if j < NTO:
            P.add("act", lambda e: e.activation(out=vv[:, j, :], in_=psv, func=AF.Copy), reads=[], writes=[PSR(b), ("v", j)])
        else:
            P.add("dve", lambda e: e.tensor_scalar(out=vv[:, j, :], in0=psv, scalar1=flagcol, scalar2=None, op0=ALU.mult),
                  reads=["cB"], writes=[PSR(b), ("v", j)])

    O1, O2, L1, L2 = 6, 7, 2, 3
    SB = ((4, 5), (0, 1))

    def att_qk(hl, gq_, i, kt):
        S1, S2 = SB[i % 2]
        c0 = (kt - 4 * gq_) * 128 if (kt < NTO and kt >= 4 * gq_) else 0
        qs = slice(gq_ * 512 + c0, (gq_ + 1) * 512)
        ksl = slice(kt * 128, (kt + 1) * 128)
        qreads = [("qT", 4 * gq_ + c) for c in range(4)]
        P.add("pe", lambda e: e.matmul(ps[S1][:, c0:512], lhsT=kT[0:64, hl, ksl], rhs=qT[0:64, hl, qs], start=True, stop=True),
              reads=[("kT", kt)] + qreads, writes=[PSR(S1)])
        P.add("pe", lambda e: e.matmul(ps[S2][:, c0:512], lhsT=kT[64:128, hl, ksl], rhs=qT[64:128, hl, qs], start=True, stop=True),
              reads=[("kT", kt)] + qreads, writes=[PSR(S2)])

    def att_pv(hl, gq_, i, kt, nk):
        S1, S2 = SB[i % 2]
        pb = i % 2
        diag = (kt < NTO and kt >= 4 * gq_)
        c0 = (kt - 4 * gq_) * 128 if diag else 0
        P.add("act", lambda e: e.activation(out=Pm[0][pb][:, c0:512], in_=ps[S1][:, c0:512], func=AF.Exp), reads=[], writes=[PSR(S1), ("P0", pb)])
        P.add("act", lambda e: e.activation(out=Pm[1][pb][:, c0:512], in_=ps[S2][:, c0:512], func=AF.Exp), reads=[], writes=[PSR(S2), ("P1", pb)])
        if diag:
            P.add("dve", lambda e: e.tensor_tensor(out=Pm[0][pb][:, c0:c0 + 128], in0=Pm[0][pb][:, c0:c0 + 128], in1=dmask_bf[:, 0, 0:128], op=ALU.mult),
                  reads=["dmask", ("P0", pb)], writes=[("P0", pb)])
            P.add("dve", lambda e: e.tensor_tensor(out=Pm[1][pb][:, c0:c0 + 128], in0=Pm[1][pb][:, c0:c0 + 128], in1=dmask_bf[:, 0, 0:128], op=ALU.mult),
                  reads=["dmask", ("P1", pb)], writes=[("P1", pb)])
        lw = flag_bf if kt >= NTO else ones_bf
        first, last = (i == 0), (i == nk - 1)
        vsl = vv[:, kt, hl * 128:(hl + 1) * 128]
        P.add("pe", lambda e: e.matmul(ps[O1][:, c0:512], lhsT=vsl, rhs=Pm[0][pb][:, c0:512], start=first, stop=last), reads=[("v", kt), ("P0", pb)], writes=[PSR(O1)])
        P.add("pe", lambda e: e.matmul(ps[L1][:, c0:512], lhsT=lw, rhs=Pm[0][pb][:, c0:512], start=first, stop=last), reads=["ones", "flagm", ("P0", pb)], writes=[PSR(L1)])
        P.add("pe", lambda e: e.matmul(ps[O2][:, c0:512], lhsT=vsl, rhs=Pm[1][pb][:, c0:512], start=first, stop=last), reads=[("v", kt), ("P1", pb)], writes=[PSR(O2)])
        P.add("pe", lambda e: e.matmul(ps[L2][:, c0:512], lhsT=lw, rhs=Pm[1][pb][:, c0:512], start=first, stop=last), reads=["ones", "flagm", ("P1", pb)], writes=[PSR(L2)])

    def att_epilogue(hglob, gq_):
        qs = slice(gq_ * 512, (gq_ + 1) * 512)
        P.add("dve", lambda e: e.reciprocal(out=rl[0], in_=ps[L1][:, :]), reads=[], writes=[PSR(L1), ("rl", 0)])
        P.add("dve", lambda e: e.tensor_tensor(out=tt[0], in0=ps[O1][:, :], in1=rl[0], op=ALU.mult), reads=[("rl", 0)], writes=[PSR(O1), ("tt", 0)])
        P.add("dve", lambda e: e.reciprocal(out=rl[1], in_=ps[L2][:, :]), reads=[], writes=[PSR(L2), ("rl", 1)])
        P.add("dve", lambda e: e.tensor_tensor(out=tt[1], in0=ps[O2][:, :], in1=rl[1], op=ALU.mult), reads=[("rl", 1)], writes=[PSR(O2), ("tt", 1)])
        P.add("dve", lambda e: e.scalar_tensor_tensor(out=ot, in0=tt[1], scalar=neglam, in1=tt[0], op0=ALU.mult, op1=ALU.add),
              reads=[("tt", 0), ("tt", 1), "neglam"], writes=["ot"])
        P.add("act", lambda e: e.activation(out=osq, in_=ot, func=AF.Square), reads=["ot"], writes=["osq"])
        P.add("pe", lambda e: e.matmul(ps[L1][:, :], lhsT=ones_bf, rhs=osq, start=True, stop=True), reads=["ones", "osq"], writes=[PSR(L1)])
        P.add("act", lambda e: e.activation(out=rstd_o, in_=ps[L1][:, :], func=AF.Ln, scale=1.0 / 128, bias=EPS), reads=[], writes=[PSR(L1), ("rl", 0)])
        P.add("act", lambda e: e.activation(out=rstd_o, in_=rstd_o, func=AF.Exp, scale=-0.5), reads=[("rl", 0)], writes=[("rl", 0)])
        P.add("dve", lambda e: e.scalar_tensor_tensor(out=mixT[:, hglob, qs], in0=ot, scalar=gout_s, in1=rstd_o, op0=ALU.mult, op1=ALU.mult),
              reads=["ot", "gout_s", ("rl", 0)], writes=[("mixT", hglob, gq_)])

    def attention(hg):
        for hl in range(4):
            for gq_ in range(2):
                emit_conv(1)
                kts = list(range(NTO, NTA)) + list(range(0, 4 * gq_ + 4))
                att_qk(hl, gq_, 0, kts[0])
                for i, kt in enumerate(kts):
                    if i + 1 < len(kts):
                        att_qk(hl, gq_, i + 1, kts[i + 1])
                    att_pv(hl, gq_, i, kt, len(kts))
                att_epilogue(hg * 4 + hl, gq_)

    blocks = []
    for hg in range(2):
        blocks.append(("q", C_DQ + hg * 512, 512, OWN, hg))
        blocks.append(("k", C_DK + hg * 512, 512, ALL, hg))
        blocks.append(("v", C_DV + hg * 512, 512, ALL, hg))
    slots = [None] * len(blocks)
    mmbanks[0] = [0, 1, 6, 7]
    slots[0] = load_w(blocks[0][1], blocks[0][2])
    for bi, (kind, c0, n, tiles, hg) in enumerate(blocks):
        if bi + 1 < len(blocks):
            slots[bi + 1] = load_w(blocks[bi + 1][1], blocks[bi + 1][2])
        LA = 3
        nst = 1 if kind == "v" else 3
        pend = [proj_tile(slots[bi], n, tiles[t]) for t in range(min(LA, len(tiles)))]
        staged = {}
        for it in range(len(tiles) + nst - 1):
            if it < len(tiles):
                j = tiles[it]
                b = pend.pop(0)
                if it + LA < len(tiles):
                    pend.append(proj_tile(slots[bi], n, tiles[it + LA]))
                if kind == "q":
                    staged[it] = qk_consumer(j, b, True, qT)
                elif kind == "k":
                    staged[it] = qk_consumer(j, b, False, kT)
                else:
                    staged[it] = [(lambda j, b: lambda: v_consumer(j, b))(j, b)]
            for sidx in range(nst):
                t = it - sidx
                if 0 <= t < len(tiles):
                    staged[t][sidx]()
        if kind == "v" and stage == "qkv":
            P.add("sp", lambda e: e.dma_start(out=g["dbgb_d"][:, 0:4096], in_=qT.rearrange("p a b -> p (a b)")),
                  reads=[("qT", j) for j in range(NTO)], writes=["dbg0"], dma=K("dbg0"))
            P.add("sp", lambda e: e.dma_start(out=g["dbgb_d"][:, 4096:12288], in_=kT.rearrange("p a b -> p (a b)")),
                  reads=[("kT", j) for j in range(NTA)], writes=["dbg1"], dma=K("dbg1"))
            P.add("sp", lambda e: e.dma_start(out=g["dbgb_d"][:, 12288:16384], in_=vv[:, 8:16, :].rearrange("p a b -> p (a b)")),
                  reads=[("v", j) for j in range(NTA)], writes=["dbg"], dma=K("dbg"))
            return
        if kind == "v":
            P.barrier(bar)
            attention(hg)
            P.barrier(bar)
            if stage == "attn0":
                break

    if stage in ("attn0", "attn"):
        P.add("sp", lambda e: e.dma_start(out=g["dbgb_d"][:, :], in_=mixT.rearrange("p a b -> p (a b)")),
              reads=[("mixT", h, q) for h in range(8) for q in range(2)], writes=["dbg"], dma=K("dbg"))
        return

    tri_f, ones_f, wg2_f = g["tri_f"], g["ones_f"], g["wg2_f"]
    mmbanks[0] = [0, 1]
    P.barrier(bar)
    RD.reset()
    gaT = RD.alloc(2048 * 4, F32)
    gkb = RD.alloc(NTA * 256 * 2, BF16, [NTA, 256])
    gvb = RD.alloc(NTA * 512 * 2, BF16, [NTA, 512])
    gqb = RD.alloc(NTO * 256 * 2, BF16, [NTO, 256])
    grb = RD.alloc(NTO * 512 * 2, BF16, [NTO, 512])
    T = {}
    for nm in ("pre", "ee", "nla", "totS", "dlt", "E1", "E2", "E3"):
        T[nm] = [RD.alloc(1024, F32)] * 2
    for nm in ("qin", "kin"):
        T[nm] = [RD.alloc(512, BF16)] * 2
    T["kdec"] = [RD.alloc(512, BF16) for _ in range(2)]
    qkT = [RD.alloc(4 * 128 * 2, BF16, [4, 128]) for _ in range(2)]
    decb = [RD.alloc(16, F32) for _ in range(2)]
    attm = RD.alloc(256, BF16)
    S_f = RD.alloc(2 * 256 * 4, F32, [2, 256])
    S_bf = RD.alloc(2 * 256 * 2, BF16, [2, 256])
    on_ = RD.alloc(1024, F32)
    eg = RD.alloc(1024, F32)
    gob = RD.alloc(512, BF16)
    ssg = RD.alloc(4, F32)
    rsg = RD.alloc(4, F32)
    bg = cB[:, CB_BG:CB_BG + 512]
    gog = cB[:, CB_GOG:CB_GOG + 256]

    def ga_block(s):
        for tg in range(4):
            b = mmbank[0] % 2
            mmbank[0] += 1
            for kc in range(KC):
                P.add("pe", (lambda kc, tg, b: lambda e: e.matmul(ps[b][0:16, :], lhsT=wbuf[s][:, kc, 0:16], rhs=hT[:, kc, tg * 512:(tg + 1) * 512],
                                                                  start=(kc == 0), stop=(kc == KC - 1)))(kc, tg, b),
                      reads=[("hT", 4 * tg + c) for c in range(4)] + [("wb", s)], writes=[PSR(b)])
            P.add("act", (lambda tg, b: lambda e: e.activation(out=gaT[0:16, tg * 512:(tg + 1) * 512], in_=ps[b][0:16, :], func=AF.Copy))(tg, b),
                  reads=[], writes=[PSR(b), ("gaT", tg)])

    def copy_consumer(dst, nm, n):
        def f(j, b):
            if j % 2 == 0:
                P.add("act", lambda e: e.activation(out=dst[:, j, :], in_=ps[b][:, 0:n], func=AF.Copy), reads=[], writes=[PSR(b), (nm, j)])
            else:
                P.add("dve", lambda e: e.tensor_copy(out=dst[:, j, :], in_=ps[b][:, 0:n]), reads=[], writes=[PSR(b), (nm, j)])
        return f

    gcnt = [0]

    def gla_tile(hp, j, own):
        pj = 0
        pq = gcnt[0] % 2
        gcnt[0] += 1
        tsl = slice(j * 128, (j + 1) * 128)
        c0 = hp * 256
        pre, ee, nla, totS, dlt = T["pre"][pj], T["ee"][pj], T["nla"][pj], T["totS"][pj], T["dlt"][pj]
        E1, E2, E3, kdec, qin, kin = T["E1"][pj], T["E2"][pj], T["E3"][pj], T["kdec"][pq], T["qin"][pj], T["kin"][pj]
        dec, qk = decb[pq], qkT[pq]
        R = lambda nm: (nm, pq) if nm in ("kdec", "qk", "dec") else (nm, pj)
        P.add("pe", lambda e: e.matmul(ps[0][:, 0:256], lhsT=gaT[0:16, tsl], rhs=wg2_f[0:16, c0:c0 + 256], start=True, stop=True),
              reads=[("gaT", j // 4), "wg2"], writes=[PSR(0)])
        P.add("dve", lambda e: e.tensor_tensor(out=pre, in0=ps[0][:, 0:256], in1=bg[:, c0:c0 + 256], op=ALU.add), reads=["cB"], writes=[PSR(0), R("pre")])
        P.add("act", lambda e: e.activation(out=ee, in_=pre, func=AF.Exp, scale=-1.0), reads=[R("pre")], writes=[R("ee")])
        P.add("act", lambda e: e.activation(out=nla, in_=ee, func=AF.Ln, bias=1.0), reads=[R("ee")], writes=[R("nla")])
        P.add("pe", lambda e: e.matmul(ps[0][:, 0:256], lhsT=tri_f, rhs=nla, start=True, stop=True), reads=["tri_f", R("nla")], writes=[PSR(0)])
        P.add("pe", lambda e: e.matmul(ps[1][:, 0:256], lhsT=ones_f, rhs=nla, start=True, stop=True), reads=["ones_f", R("nla")], writes=[PSR(1)])
        for hh in range(2):
            P.add("pe", (lambda hh: lambda e: e.matmul(ps[2][:, 2 * hh:2 * hh + 2], lhsT=nla[:, hh * 128:(hh + 1) * 128], rhs=ones_f[:, 0:2],
                                                       start=True, stop=True))(hh), reads=["ones_f", R("nla")], writes=[PSR(2)])
        P.add("act", lambda e: e.activation(out=dec, in_=ps[2][:, 0:4], func=AF.Exp, scale=-1.0 / 16), reads=[], writes=[PSR(2), R("dec")])
        P.add("act", lambda e: e.activation(out=totS, in_=ps[1][:, 0:256], func=AF.Copy), reads=[], writes=[PSR(1), R("totS")])
        P.add("dve", lambda e: e.tensor_tensor(out=dlt, in0=ps[0][:, 0:256], in1=totS, op=ALU.subtract), reads=[R("totS")], writes=[PSR(0), R("dlt")])
        P.add("act", lambda e: e.activation(out=E3, in_=dlt, func=AF.Exp, scale=1.0 / 16), reads=[R("dlt")], writes=[R("E3")])
        P.add("dve", lambda e: e.tensor_tensor(out=kdec, in0=gkb[:, j, :], in1=E3, op=ALU.mult), reads=[("gk", j), R("E3")], writes=[R("kdec")])
        if own:
            P.add("act", lambda e: e.activation(out=E1, in_=ps[0][:, 0:256], func=AF.Exp, scale=-1.0 / 16), reads=[], writes=[PSR(0), R("E1")])
            P.add("act", lambda e: e.activation(out=E2, in_=ps[0][:, 0:256], func=AF.Exp, scale=1.0 / 16), reads=[], writes=[PSR(0), R("E2")])
            P.add("dve", lambda e: e.scalar_tensor_tensor(out=qin, in0=gqb[:, j, :], scalar=float(128 ** -0.5), in1=E1, op0=ALU.mult, op1=ALU.mult),
                  reads=[("gq", j), R("E1")], writes=[R("qin")])
            P.add("dve", lambda e: e.tensor_tensor(out=kin, in0=gkb[:, j, :], in1=E2, op=ALU.mult), reads=[("gk", j), R("E2")], writes=[R("kin")])
            for hh in range(2):
                P.add("pe", (lambda hh: lambda e: e.transpose(out=psb[3][:, hh * 128:(hh + 1) * 128], in_=qin[:, hh * 128:(hh + 1) * 128],
                                                              identity=ident_bf))(hh), reads=[R("qin"), "ident"], writes=[PSR(3)])
            for hh in range(2):
                P.add("pe", (lambda hh: lambda e: e.transpose(out=psb[3][:, 256 + hh * 128:256 + (hh + 1) * 128], in_=kin[:, hh * 128:(hh + 1) * 128],
                                                              identity=ident_bf))(hh), reads=[R("kin"), "ident"], writes=[PSR(3)])
            P.add("act", lambda e: e.activation(out=qk, in_=psb[3][:, 0:512].rearrange("p (a b) -> p a b", b=128), func=AF.Copy),
                  reads=[], writes=[PSR(3), R("qk")])
        def heads():
            for hh in range(2):
                gla_head(hp, j, own, hh, kdec, qk, dec, pq)
        return heads

    def gla_head(hp, j, own, hh, kdec, qk, dec, pq):
        tsl = slice(j * 128, (j + 1) * 128)
        R = lambda nm: (nm, pq)
        vsl = gvb[:, j, hh * 256:(hh + 1) * 256]
        if own:
            P.add("pe", lambda e: e.matmul(ps[4][:, 0:128], lhsT=qk[:, 2 + hh, :], rhs=qk[:, hh, :], start=True, stop=True), reads=[R("qk")], writes=[PSR(4)])
            P.add("dve", lambda e: e.tensor_tensor(out=attm, in0=ps[4][:, 0:128], in1=tri_f, op=ALU.mult), reads=["tri_f"], writes=[PSR(4), "attm"])
            P.add("pe", lambda e: e.matmul(ps[5][:, 0:256], lhsT=attm, rhs=vsl, start=True, stop=False), reads=["attm", ("gv", j)], writes=[PSR(5)])
            P.add("pe", lambda e: e.matmul(ps[5][:, 0:256], lhsT=qk[:, hh, :], rhs=S_bf[:, hh, :], start=False, stop=True), reads=[R("qk"), ("S_bf", hh)], writes=[PSR(5)])
            P.add("act", lambda e: e.activation(out=eg, in_=ps[5][:, 0:256], func=AF.Square, accum_out=ssg), reads=[], writes=[PSR(5), "eg", "ssg"])
            P.add("act", lambda e: e.activation(out=rsg, in_=ssg, func=AF.Ln, scale=1.0 / 256, bias=EPS), reads=["ssg"], writes=["rsg"])
            P.add("act", lambda e: e.activation(out=rsg, in_=rsg, func=AF.Exp, scale=-0.5), reads=["rsg"], writes=["rsg"])
            P.add("dve", lambda e: e.scalar_tensor_tensor(out=on_, in0=ps[5][:, 0:256], scalar=rsg, in1=gog, op0=ALU.mult, op1=ALU.mult),
                  reads=["rsg", "cB"], writes=[PSR(5), "on"])
            gsl = grb[:, j, hh * 256:(hh + 1) * 256]
            P.add("act", lambda e: e.activation(out=eg, in_=gsl, func=AF.Exp, scale=-1.0), reads=[("gr", j)], writes=["eg"])
            P.add("dve", lambda e: e.tensor_scalar_add(out=eg, in0=eg, scalar1=1.0), reads=["eg"], writes=["eg"])
            P.add("dve", lambda e: e.reciprocal(out=eg, in_=eg), reads=["eg"], writes=["eg"])
            P.add("dve", lambda e: e.tensor_tensor(out=on_, in0=on_, in1=eg, op=ALU.mult), reads=["on", "eg"], writes=["on"])
            P.add("dve", lambda e: e.tensor_tensor(out=gob, in0=on_, in1=gsl, op=ALU.mult), reads=["on", ("gr", j)], writes=["gob"])
            for c in range(2):
                P.add("pe", (lambda c: lambda e: e.transpose(out=psb[6][:, c * 128:(c + 1) * 128], in_=gob[:, c * 128:(c + 1) * 128], identity=ident_bf))(c),
                      reads=["gob", "ident"], writes=[PSR(6)])
            kc0 = 8 + 2 * (2 * hp + hh)
            P.add("act", lambda e: e.activation(out=mixT[:, kc0:kc0 + 2, tsl], in_=psb[6][:, 0:256].rearrange("p (a b) -> p a b", b=128), func=AF.Copy),
                  reads=[], writes=[PSR(6), ("mixTg", kc0, j)])
        P.add("pe", lambda e: e.matmul(ps[7][:, 0:256], lhsT=kdec[:, hh * 128:(hh + 1) * 128], rhs=vsl, start=True, stop=True),
              reads=[R("kdec"), ("gv", j)], writes=[PSR(7)])
        P.add("dve", lambda e: e.scalar_tensor_tensor(out=S_f[:, hh, :], in0=S_f[:, hh, :], scalar=dec[:, 2 * hh:2 * hh + 1], in1=ps[7][:, 0:256],
                                                      op0=ALU.mult, op1=ALU.add), reads=[R("dec"), ("S_f", hh)], writes=[PSR(7), ("S_f", hh)])
        P.add("act", lambda e: e.activation(out=S_bf[:, hh, :], in_=S_f[:, hh, :], func=AF.Copy), reads=[("S_f", hh)], writes=[("S_bf", hh)])

    def gla_scan(hp):
        P.add("dve", lambda e: e.memset(S_f.rearrange("p a b -> p (a b)"), 0.0), reads=[], writes=[("S_f", 0), ("S_f", 1)])
        P.add("dve", lambda e: e.memset(S_bf.rearrange("p a b -> p (a b)"), 0.0), reads=[], writes=[("S_bf", 0), ("S_bf", 1)])
        order = [(j, False) for j in range(NTO, NTA)] + [(j, True) for j in range(NTO)]

        def capture(fn):
            old_ops = P.ops
            P.ops = []
            ret = fn()
            got = P.ops
            P.ops = old_ops
            return got, ret

        def merge(a, b):
            out, ia, ib = [], 0, 0
            while ia < len(a) or ib < len(b):
                if ib >= len(b) or (ia < len(a) and ia * len(b) <= ib * len(a)):
                    out.append(a[ia]); ia += 1
                else:
                    out.append(b[ib]); ib += 1
            return out

        nxt = gla_tile(hp, order[0][0], order[0][1])
        for i, (j, own) in enumerate(order):
            if i % 2 == 0:
                emit_conv(1)
            cur = nxt
            pre_ops = []
            if i + 1 < len(order):
                pre_ops, nxt = capture(lambda: gla_tile(hp, order[i + 1][0], order[i + 1][1]))
            if own and j == 0:
                P.add("dve", lambda e: e.tensor_scalar(out=S_f.rearrange("p a b -> p (a b)"), in0=S_f.rearrange("p a b -> p (a b)"), scalar1=flagcol, scalar2=None, op0=ALU.mult),
                      reads=["cB", ("S_f", 0), ("S_f", 1)], writes=[("S_f", 0), ("S_f", 1)])
                P.add("act", lambda e: e.activation(out=S_bf.rearrange("p a b -> p (a b)"), in_=S_f.rearrange("p a b -> p (a b)"), func=AF.Copy),
                      reads=[("S_f", 0), ("S_f", 1)], writes=[("S_bf", 0), ("S_bf", 1)])
            head_ops, _ = capture(cur)
            P.ops.extend(merge(pre_ops, head_ops))

    gblocks = [("ga", C_GA, 16, None, None)]
    for hp in range(2):
        gblocks.append(("gq", C_GQ + hp * 256, 256, OWN, hp))
        gblocks.append(("gk", C_GK + hp * 256, 256, ALL, hp))
        gblocks.append(("gv", C_GV + hp * 512, 512, ALL, hp))
        gblocks.append(("gr", C_GR + hp * 512, 512, OWN, hp))
    cons = dict(gq=copy_consumer(gqb, "gq", 256), gk=copy_consumer(gkb, "gk", 256), gv=copy_consumer(gvb, "gv", 512), gr=copy_consumer(grb, "gr", 512))
    gslots = [None] * len(gblocks)
    gslots[0] = load_w(gblocks[0][1], gblocks[0][2])
    for bi, (kind, c0, n, tiles, hp) in enumerate(gblocks):
        if bi + 1 < len(gblocks):
            gslots[bi + 1] = load_w(gblocks[bi + 1][1], gblocks[bi + 1][2])
        if kind == "ga":
            ga_block(gslots[bi])
            continue
        for j in tiles:
            b = proj_tile(gslots[bi], n, j)
            cons[kind](j, b)
        if kind == "gr":
            gla_scan(hp)

    if stage == "gla":
        P.add("sp", lambda e: e.dma_start(out=g["dbgb_d"][:, :], in_=mixT.rearrange("p a b -> p (a b)")),
              reads=[("mixT", h, q) for h in range(8) for q in range(2)] + [("mixTg", 8 + 2 * h, j) for h in range(4) for j in range(NTO)],
              writes=["dbg"], dma=K("dbg"))
        return

    RA, RB, RC, RE = g["RA"], g["RB"], g["RC"], g["RE"]
    w_out_v, x_own, x1_d, xg_d, yb_d, out_d, g2_d = (g[k] for k in ("w_out_v", "x_own", "x1_d", "xg_d", "yb_d", "out_d", "g2_d"))
    trs_bf, wr_bf = g["trs_bf"], g["wr_bf"]
    wout = hT
    mix_res = [("mixT", h, q) for h in range(8) for q in range(2)] + [("mixTg", 8 + 2 * h, j) for h in range(4) for j in range(NTO)]
    for cb in range(4):
        P.add("pool", (lambda cb: lambda e: e.dma_start(out=wout[:, :, cb * 512:(cb + 1) * 512], in_=w_out_v[:, :, cb * 512:(cb + 1) * 512]))(cb),
              writes=[("hT", j) for j in ALL] + [("wout", cb)], dma=K(("wout", cb)), nophase=True)
    pos1s = RE.alloc(NTO * 4, I32)
    pos2s = RE.alloc(NTO * 4, I32)
    pos1g = RE.alloc(NTO * 4, I32)
    pos2g = RE.alloc(NTO * 4, I32)
    cw1 = RE.alloc(NTO * 4, F32)
    cw2 = RE.alloc(NTO * 4, F32)
    P.barrier(bar)
    RD.reset()
    xr = [RD.alloc(DM * 4, F32) for _ in range(2)]
    x1 = [RD.alloc(DM * 4, F32) for _ in range(2)]
    g2 = RD.alloc(DM * 4, F32)
    h2b = [RD.alloc(DM * 2, BF16) for _ in range(2)]
    h2T = RD.alloc(KC * 128 * 2, BF16, [KC, 128])
    sq2 = RD.alloc(DM * 2, BF16)
    Rb = RD.alloc(NTO * 32 * 2, BF16, [NTO, 32])
    ss2 = RD.alloc(4, F32)
    rs2 = RD.alloc(4, F32)
    lg = RD.alloc(36 * 4, F32)
    W = {}
    for nm, n in (("mxg", 1), ("ohg", 4), ("negm", 1), ("eg4", 4), ("sg", 1), ("pg", 1), ("tmp", 32), ("les", 8), ("m1", 1), ("oh1", 8),
                  ("msk", 8), ("m2", 1), ("oh2", 8), ("dm", 1), ("ed", 1), ("w1", 1), ("w2", 1), ("R1", 32), ("R2", 32), ("pr", 32),
                  ("vl", 32), ("t32", 32), ("p1", 1), ("v1", 1), ("p2", 1), ("v2", 1), ("tA", 1), ("tB", 1)):
        W[nm] = RD.alloc(n * 4, F32)
    rbias = cB[:, CB_RB:CB_RB + 36]
    pbase = cB[:, CB_PB:CB_PB + 32]
    P.add("sp", lambda e: e.dma_start(out=g2, in_=g2_d[:, :]), writes=["g2"], dma=K("g2"))
    emit_conv(100)
    XGD = [("xgd", u_) for u_ in range(NE * CAP // 128)]
    BIGI = 20000.0

    def D(fn, reads, writes):
        P.add("dve", fn, reads=reads, writes=writes)

    def outproj_tile(j):
        pj = j % 2
        rsl = slice(j * 128, (j + 1) * 128)
        P.add("sp", lambda e: e.dma_start(out=xr[pj], in_=x_own[rsl, :]), writes=[("xr", pj)], dma=K(("xr", pj)))
        for cb in range(4):
            b = mmbank[0] % 2
            mmbank[0] += 1
            csl = slice(cb * 512, (cb + 1) * 512)
            for kc in range(KC):
                P.add("pe", (lambda kc, b, csl: lambda e: e.matmul(ps[b][:, :], lhsT=mixT[:, kc, rsl], rhs=wout[:, kc, csl], start=(kc == 0), stop=(kc == KC - 1)))(kc, b, csl),
                      reads=mix_res + [("wout", cb)], writes=[PSR(b)])
            P.add("dve", (lambda b, csl: lambda e: e.tensor_tensor(out=x1[pj][:, csl], in0=ps[b][:, :], in1=xr[pj][:, csl], op=ALU.add))(b, csl),
                  reads=[("xr", pj)], writes=[PSR(b), ("x1", pj, cb)])

    def dispatch_tile(j):
        pj = j % 2
        rsl = slice(j * 128, (j + 1) * 128)
        x1r = [("x1", pj, cb) for cb in range(4)]
        P.add("sp", lambda e: e.dma_start(out=x1_d[rsl, :], in_=x1[pj]), reads=x1r, writes=[("x1d", j)], dma=K(("x1s", pj)))
        P.add("act", lambda e: e.activation(out=sq2, in_=x1[pj], func=AF.Square, accum_out=ss2), reads=x1r, writes=["sq2", "ss2"])
        P.add("act", lambda e: e.activation(out=rs2, in_=ss2, func=AF.Ln, scale=1.0 / DM, bias=EPS), reads=["ss2"], writes=["rs2"])
        P.add("act", lambda e: e.activation(out=rs2, in_=rs2, func=AF.Exp, scale=-0.5), reads=["rs2"], writes=["rs2"])
        D(lambda e: e.scalar_tensor_tensor(out=h2b[pj], in0=x1[pj], scalar=rs2, in1=g2, op0=ALU.mult, op1=ALU.mult), x1r + ["rs2", "g2"], [("h2b", pj)])
        for half in range(2):
            bank = 2 + half
            for i in range(8):
                kc = half * 8 + i
                P.add("pe", (lambda kc, i, bank: lambda e: e.transpose(out=psb[bank][:, i * 128:(i + 1) * 128], in_=h2b[pj][:, kc * 128:(kc + 1) * 128],
                                                                       identity=ident_bf))(kc, i, bank), reads=[("h2b", pj), "ident"], writes=[PSR(bank)])
            if half == 0:
                P.add("act", lambda e: e.activation(out=h2T[:, 0:8, :], in_=psb[2].rearrange("p (a b) -> p a b", b=128), func=AF.Copy), reads=[], writes=[PSR(2), "h2Ta"])
            else:
                D(lambda e: e.tensor_copy(out=h2T[:, 8:16, :], in_=psb[3].rearrange("p (a b) -> p a b", b=128)), [], [PSR(3), "h2Tb"])
        for kc in range(KC):
            P.add("pe", (lambda kc: lambda e: e.matmul(ps[4][:, 0:36], lhsT=h2T[:, kc, :], rhs=wr_bf[:, kc, :], start=(kc == 0), stop=(kc == KC - 1)))(kc),
                  reads=["h2Ta", "h2Tb", "wr"], writes=[PSR(4)])
        D(lambda e: e.tensor_tensor(out=lg, in0=ps[4][:, 0:36], in1=rbias, op=ALU.add), ["cB"], [PSR(4), "lg"])
        lgg = lg[:, 0:4]
        le = lg[:, 4:36].rearrange("p (a b) -> p a b", b=8)
        D(lambda e: e.tensor_reduce(out=W["mxg"], in_=lgg, axis=AX.X, op=ALU.max), ["lg"], ["mxg"])
        D(lambda e: e.tensor_scalar(out=W["ohg"], in0=lgg, scalar1=W["mxg"], scalar2=None, op0=ALU.is_equal), ["lg", "mxg"], ["ohg"])
        D(lambda e: e.tensor_scalar_mul(out=W["negm"], in0=W["mxg"], scalar1=-1.0), ["mxg"], ["negm"])
        P.add("act", lambda e: e.activation(out=W["eg4"], in_=lgg, func=AF.Exp, bias=W["negm"], accum_out=W["sg"]), reads=["lg", "negm"], writes=["eg4", "sg"])
        D(lambda e: e.reciprocal(out=W["pg"], in_=W["sg"]), ["sg"], ["pg"])
        t3 = W["tmp"].rearrange("p (a b) -> p a b", b=8)
        D(lambda e: e.tensor_tensor(out=t3, in0=le, in1=W["ohg"].unsqueeze(2).to_broadcast([128, 4, 8]), op=ALU.mult), ["lg", "ohg"], ["tmp"])
        D(lambda e: e.tensor_reduce(out=W["les"], in_=t3.rearrange("p a b -> p b a"), axis=AX.X, op=ALU.add), ["tmp"], ["les"])
        D(lambda e: e.tensor_reduce(out=W["m1"], in_=W["les"], axis=AX.X, op=ALU.max), ["les"], ["m1"])
        D(lambda e: e.tensor_scalar(out=W["oh1"], in0=W["les"], scalar1=W["m1"], scalar2=None, op0=ALU.is_equal), ["les", "m1"], ["oh1"])
        D(lambda e: e.scalar_tensor_tensor(out=W["msk"], in0=W["oh1"], scalar=-1e30, in1=W["les"], op0=ALU.mult, op1=ALU.add), ["oh1", "les"], ["msk"])
        D(lambda e: e.tensor_reduce(out=W["m2"], in_=W["msk"], axis=AX.X, op=ALU.max), ["msk"], ["m2"])
        D(lambda e: e.tensor_scalar(out=W["oh2"], in0=W["msk"], scalar1=W["m2"], scalar2=None, op0=ALU.is_equal), ["msk", "m2"], ["oh2"])
        D(lambda e: e.tensor_tensor(out=W["dm"], in0=W["m2"], in1=W["m1"], op=ALU.subtract), ["m1", "m2"], ["dm"])
        P.add("act", lambda e: e.activation(out=W["ed"], in_=W["dm"], func=AF.Exp), reads=["dm"], writes=["ed"])
        D(lambda e: e.tensor_scalar_add(out=W["w1"], in0=W["ed"], scalar1=1.0), ["ed"], ["w1"])
        D(lambda e: e.reciprocal(out=W["w1"], in_=W["w1"]), ["w1"], ["w1"])
        D(lambda e: e.tensor_tensor(out=W["w2"], in0=W["ed"], in1=W["w1"], op=ALU.mult), ["ed", "w1"], ["w2"])
        R1 = W["R1"].rearrange("p (a b) -> p a b", b=8)
        R2 = W["R2"].rearrange("p (a b) -> p a b", b=8)
        ohgb = W["ohg"].unsqueeze(2).to_broadcast([128, 4, 8])
        D(lambda e: e.tensor_tensor(out=R1, in0=ohgb, in1=W["oh1"].unsqueeze(1).to_broadcast([128, 4, 8]), op=ALU.mult), ["ohg", "oh1"], ["R1"])
        D(lambda e: e.tensor_tensor(out=R2, in0=ohgb, in1=W["oh2"].unsqueeze(1).to_broadcast([128, 4, 8]), op=ALU.mult), ["ohg", "oh2"], ["R2"])
        D(lambda e: e.tensor_tensor(out=Rb[:, j, :], in0=W["R1"], in1=W["R2"], op=ALU.add), ["R1", "R2"], [("Rb", j)])
        P.add("pe", lambda e: e.matmul(ps[5][:, 0:32], lhsT=trs_bf, rhs=Rb[:, j, :], start=True, stop=(j == 0)), reads=["trs", ("Rb", j)], writes=[PSR(5)])
        for i in range(j):
            P.add("pe", (lambda i: lambda e: e.matmul(ps[5][:, 0:32], lhsT=ones_bf, rhs=Rb[:, i, :], start=False, stop=(i == j - 1)))(i),
                  reads=["ones", ("Rb", i)], writes=[PSR(5)])
        D(lambda e: e.tensor_tensor(out=W["pr"], in0=ps[5][:, 0:32], in1=pbase, op=ALU.add), ["cB"], [PSR(5), "pr"])
        D(lambda e: e.tensor_single_scalar(out=W["vl"], in_=ps[5][:, 0:32], scalar=float(CAP), op=ALU.is_lt), [], [PSR(5), "vl"])
        for (Rk, pk, vk, cwk, posS, posG, wk) in (("R1", "p1", "v1", cw1, pos1s, pos1g, "w1"), ("R2", "p2", "v2", cw2, pos2s, pos2g, "w2")):
            def one(Rk=Rk, pk=pk, vk=vk, cwk=cwk, posS=posS, posG=posG, wk=wk):
                D(lambda e: e.tensor_tensor(out=W["t32"], in0=W[Rk], in1=W["pr"], op=ALU.mult), [Rk, "pr"], ["t32"])
                D(lambda e: e.tensor_reduce(out=W[pk], in_=W["t32"], axis=AX.X, op=ALU.add), ["t32"], [pk])
                D(lambda e: e.tensor_tensor(out=W["t32"], in0=W[Rk], in1=W["vl"], op=ALU.mult), [Rk, "vl", pk], ["t32"])
                D(lambda e: e.tensor_reduce(out=W[vk], in_=W["t32"], axis=AX.X, op=ALU.add), ["t32"], [vk])
                D(lambda e: e.tensor_scalar_add(out=W["tA"], in0=W[pk], scalar1=-BIGI), [pk], ["tA"])
                D(lambda e: e.tensor_tensor(out=W["tA"], in0=W["tA"], in1=W[vk], op=ALU.mult), ["tA", vk], ["tA"])
                D(lambda e: e.tensor_scalar_add(out=W["tA"], in0=W["tA"], scalar1=BIGI), ["tA"], ["tA"])
                D(lambda e: e.tensor_copy(out=posS[:, j:j + 1], in_=W["tA"]), ["tA"], [("posS", pk, j)])
                D(lambda e: e.tensor_tensor(out=W["tB"], in0=W[pk], in1=W[vk], op=ALU.mult), [pk, vk], ["tB"])
                D(lambda e: e.tensor_copy(out=posG[:, j:j + 1], in_=W["tB"]), ["tB"], [("posG", pk, j)])
                D(lambda e: e.tensor_tensor(out=W["tB"], in0=W[wk], in1=W[vk], op=ALU.mult), [wk, vk, ("posG", pk, j)], ["tB"])
                D(lambda e: e.tensor_tensor(out=cwk[:, j:j + 1], in0=W["tB"], in1=W["pg"], op=ALU.mult), ["tB", "pg"], [("cw", pk, j)])
                P.add("pool", lambda e: e.indirect_dma_start(out=xg_d[:, :], out_offset=bass.IndirectOffsetOnAxis(ap=posS[:, j:j + 1], axis=0),
                                                             in_=h2b[pj][:, :], in_offset=None, bounds_check=NE * CAP - 1, oob_is_err=False),
                      reads=[("h2b", pj), ("posS", pk, j)], writes=XGD, dma=K(("sc", pj, pk)))
            one()

    outproj_tile(0)
    for j in range(NTO):
        a_ops = []
        if j + 1 < NTO:
            a_ops, _ = P.capture(lambda: outproj_tile(j + 1))
        b_ops, _ = P.capture(lambda: dispatch_tile(j))
        P.ops.extend(Prog.merge(a_ops, b_ops))

    if stage == "disp":
        RC.reset()
        d1 = RC.alloc(4096, F32)
        P.add("dve", lambda e: e.tensor_copy(out=d1[:, 0:8], in_=pos1s), reads=[("posS", "p1", j) for j in range(NTO)], writes=["d1a"])
        P.add("dve", lambda e: e.tensor_copy(out=d1[:, 8:16], in_=pos2s), reads=[("posS", "p2", j) for j in range(NTO)], writes=["d1b"])
        P.add("dve", lambda e: e.tensor_copy(out=d1[:, 16:24], in_=cw1), reads=[("cw", "p1", j) for j in range(NTO)], writes=["d1c"])
        P.add("dve", lambda e: e.tensor_copy(out=d1[:, 24:32], in_=cw2), reads=[("cw", "p2", j) for j in range(NTO)], writes=["d1d"])
        P.add("sp", lambda e: e.dma_start(out=g["dbg_d"][:, 2048:4096], in_=x1[1]), reads=[("x1", 1, cb) for cb in range(4)], writes=["dbgx"], dma=K("dbgx"))
        P.add("sp", lambda e: e.dma_start(out=g["dbg_d"][:, 4096:4132], in_=lg), reads=["lg"], writes=["dbgy"], dma=K("dbgy"))
        P.add("sp", lambda e: e.dma_start(out=g["dbgb_d"][:, 0:2048], in_=h2b[1]), reads=[("h2b", 1)], writes=["dbgz"], dma=K("dbgz"))
        P.add("sp", lambda e: e.dma_start(out=g["dbgb_d"][:, 2048:4096], in_=wout[:, 3, :]), reads=[("wout", cb) for cb in range(4)], writes=["dbgw"], dma=K("dbgw"))
        P.add("sp", lambda e: e.dma_start(out=g["dbgb_d"][:, 4096:5120], in_=mixT[:, 9, :]), reads=mix_res, writes=["dbgm"], dma=K("dbgm"))
        P.add("sp", lambda e: e.dma_start(out=g["dbg_d"][:, 4200:4200 + 2048], in_=xr[1]), reads=[("xr", 1)], writes=["dbgr"], dma=K("dbgr"))
        P.add("sp", lambda e: e.dma_start(out=g["dbg_d"][:, 0:32], in_=d1[:, 0:32]), reads=["dbgx", "dbgy", "dbgz", "dbgw", "dbgm", "dbgr", "d1a", "d1b", "d1c", "d1d"] + [("x1d", j) for j in range(NTO)] + XGD,
              writes=["dbg"], dma=K("dbg"))
        return

    w_gate, w_up, w_down = g["w_gate"], g["w_up"], g["w_down"]
    P.barrier(bar)
    RD.reset()
    RA.reset()
    big = RA.big
    wslots = []
    for s_ in range(2):
        base = s_ * 48 * 1024
        wg_ = big[:, base:base + 16384].bitcast(BF16).rearrange("p (a b) -> p a b", b=FF)
        wu_ = big[:, base + 16384:base + 32768].bitcast(BF16).rearrange("p (a b) -> p a b", b=FF)
        wd_ = big[:, base + 32768:base + 49152].bitcast(BF16).rearrange("p (a b) -> p a b", b=DM)
        wslots.append((wg_, wu_, wd_))
    xg = [RD.alloc(DM * 2, BF16) for _ in range(2)]
    xgT = [RD.alloc(KC * 128 * 2, BF16, [KC, 128]) for _ in range(2)]
    sgt = RD.alloc(FF * 4, F32)
    hg = RD.alloc(FF * 2, BF16)
    hgT = RD.alloc(4 * 128 * 2, BF16, [4, 128])
    Ye = [RD.alloc(DM * 2, BF16) for _ in range(2)]
    old_w = [("wout", cb) for cb in range(4)] + [("wb", 0), ("wb", 1)] + [("hT", j) for j in ALL]

    NSTG = 8
    RC.reset()
    stg = [RC.alloc(DM * 4, F32) for _ in range(4)] + [RD.alloc(DM * 4, F32) for _ in range(4)]
    NCHUNK = NE * 12

    NCONV, wcb_d = g["NCONV"], g["wcb_d"]
    items = []
    for e_ in range(NE):
        s_ = e_ % 2
        wg_, wu_, wd_ = wslots[s_]
        if e_ >= NE - NCONV:
            m0 = 3 * (e_ - (NE - NCONV))
            items.append(dict(kind="direct", e=e_, src=wcb_d[m0].rearrange("p (a b) -> p a b", b=FF), dst=wg_, res=[("wg", s_, c) for c in range(4)], conv=m0))
            items.append(dict(kind="direct", e=e_, src=wcb_d[m0 + 1].rearrange("p (a b) -> p a b", b=FF), dst=wu_, res=[("wu", s_, c) for c in range(4)], conv=m0 + 1))
            items.append(dict(kind="direct", e=e_, src=wcb_d[m0 + 2].rearrange("p (a b) -> p a b", b=DM), dst=wd_, res=[("wd", s_, c) for c in range(4)], conv=m0 + 2))
        else:
            for c in range(4):
                items.append(dict(kind="staged", e=e_, src=w_gate[e_].rearrange("(kc p) f -> p kc f", p=128)[:, 4 * c:4 * c + 4, :], dst=wg_[:, 4 * c:4 * c + 4, :], res=[("wg", s_, c)], is3=True))
            for c in range(4):
                items.append(dict(kind="staged", e=e_, src=w_up[e_].rearrange("(kc p) f -> p kc f", p=128)[:, 4 * c:4 * c + 4, :], dst=wu_[:, 4 * c:4 * c + 4, :], res=[("wu", s_, c)], is3=True))
            for c in range(4):
                items.append(dict(kind="staged", e=e_, src=w_down[e_].rearrange("(fc p) d -> p fc d", p=128)[:, c, :], dst=wd_[:, c, :], res=[("wd", s_, c)], is3=False))
    st_ = dict(nd=0, ring_used=0, nstaged=0, ncast=0)

    def pump(max_e):
        while st_["nd"] < len(items):
            it = items[st_["nd"]]
            if it["e"] > max_e or (it["kind"] == "direct" and it["e"] > max_e - 1):
                return
            if it["kind"] == "staged":
                if st_["ring_used"] >= NSTG:
                    return
                k = st_["nstaged"] % NSTG
                st_["nstaged"] += 1
                st_["ring_used"] += 1
                it["slot"] = k
                sv = stg[k].rearrange("p (a b) -> p a b", b=FF) if it["is3"] else stg[k]
                it["sv"] = sv
                P.add("sp", (lambda sv, src: lambda e: e.dma_start(out=sv, in_=src))(sv, it["src"]), writes=[("stg", k)], dma=K(("stg", k)))
            else:
                P.add("sp", (lambda dst, src: lambda e: e.dma_start(out=dst, in_=src))(it["dst"], it["src"]), reads=[("conv", it["conv"]), ("conv", 3 * NCONV - 1)],
                      writes=it["res"], dma=K(("wdir", it["e"] % 2, it["conv"] % 3)))
            st_["nd"] += 1

    def cast_items(e_):
        pump(e_ + 1)
        for it in items:
            if it["e"] != e_ or it["kind"] != "staged":
                continue
            k, sv, dst, res = it["slot"], it["sv"], it["dst"], it["res"]
            if st_["ncast"] % 2 == 0:
                P.add("act", (lambda dst, sv: lambda e: e.activation(out=dst, in_=sv, func=AF.Copy))(dst, sv), reads=[("stg", k)], writes=res)
            else:
                P.add("dve", (lambda dst, sv: lambda e: e.tensor_copy(out=dst, in_=sv))(dst, sv), reads=[("stg", k)], writes=res)
            st_["ncast"] += 1
            st_["ring_used"] -= 1
            pump(e_ + 1)

    def load_xg(e_, st, u):
        pu = u % 2
        r0 = e_ * CAP + st * 128
        P.add("act", lambda e: e.dma_start(out=xg[pu], in_=xg_d[r0:r0 + 128, :]), reads=XGD, writes=[("xg", pu)], dma=K(("xg", pu)))

    def expert_unit(e_, st, u, part):
        s_ = e_ % 2
        pu = u % 2
        wg_, wu_, wd_ = wslots[s_]
        r0 = e_ * CAP + st * 128
        if part == "front":
            unit_front(s_, pu, wg_, wu_)
        elif part == "b1":
            P.add("act", lambda e: e.activation(out=sgt, in_=ps[0][:, :], func=AF.Silu), reads=[], writes=[PSR(0), "sgt"])
            P.add("dve", lambda e: e.tensor_tensor(out=hg, in0=ps[1][:, :], in1=sgt, op=ALU.mult), reads=["sgt"], writes=[PSR(1), "hg"])
        else:
            unit_back(s_, pu, wd_, r0, u)

    def unit_front(s_, pu, wg_, wu_):
        for half in range(2):
            bank = 2 + half
            for i in range(8):
                kc = half * 8 + i
                P.add("pe", (lambda kc, i, bank: lambda e: e.transpose(out=psb[bank][:, i * 128:(i + 1) * 128], in_=xg[pu][:, kc * 128:(kc + 1) * 128],
                                                                       identity=ident_bf))(kc, i, bank), reads=[("xg", pu), "ident"], writes=[PSR(bank)])
            if half == 0:
                P.add("act", lambda e: e.activation(out=xgT[pu][:, 0:8, :], in_=psb[2].rearrange("p (a b) -> p a b", b=128), func=AF.Copy),
                      reads=[], writes=[PSR(2), ("xgTa", pu)])
            else:
                P.add("dve", lambda e: e.tensor_copy(out=xgT[pu][:, 8:16, :], in_=psb[3].rearrange("p (a b) -> p a b", b=128)),
                      reads=[], writes=[PSR(3), ("xgTb", pu)])
        for kc in range(KC):
            P.add("pe", (lambda kc: lambda e: e.matmul(ps[0][:, :], lhsT=xgT[pu][:, kc, :], rhs=wg_[:, kc, :], start=(kc == 0), stop=(kc == KC - 1)))(kc),
                  reads=[("xgTa", pu), ("xgTb", pu), ("wg", s_, kc // 4)], writes=[PSR(0)])
        for kc in range(KC):
            P.add("pe", (lambda kc: lambda e: e.matmul(ps[1][:, :], lhsT=xgT[pu][:, kc, :], rhs=wu_[:, kc, :], start=(kc == 0), stop=(kc == KC - 1)))(kc),
                  reads=[("xgTa", pu), ("xgTb", pu), ("wu", s_, kc // 4)], writes=[PSR(1)])

    def unit_back(s_, pu, wd_, r0, u):
        for fc in range(4):
            P.add("pe", (lambda fc: lambda e: e.transpose(out=psb[4][:, fc * 128:(fc + 1) * 128], in_=hg[:, fc * 128:(fc + 1) * 128], identity=ident_bf))(fc),
                  reads=["hg", "ident"], writes=[PSR(4)])
        P.add("act", lambda e: e.activation(out=hgT, in_=psb[4][:, 0:512].rearrange("p (a b) -> p a b", b=128), func=AF.Copy), reads=[], writes=[PSR(4), "hgT"])
        for db in range(4):
            bank = 5 + db % 2
            for fc in range(4):
                P.add("pe", (lambda fc, db, bank: lambda e: e.matmul(ps[bank][:, :], lhsT=hgT[:, fc, :], rhs=wd_[:, fc, db * 512:(db + 1) * 512],
                                                                     start=(fc == 0), stop=(fc == 3)))(fc, db, bank), reads=["hgT", ("wd", s_, fc)], writes=[PSR(bank)])
            if db % 2 == 0:
                P.add("act", (lambda db, bank: lambda e: e.activation(out=Ye[pu][:, db * 512:(db + 1) * 512], in_=ps[bank][:, :], func=AF.Copy))(db, bank),
                      reads=[], writes=[PSR(bank), ("Ye", pu, db)])
            else:
                P.add("dve", (lambda db, bank: lambda e: e.tensor_copy(out=Ye[pu][:, db * 512:(db + 1) * 512], in_=ps[bank][:, :]))(db, bank),
                      reads=[], writes=[PSR(bank), ("Ye", pu, db)])
        P.add("act", lambda e: e.dma_start(out=yb_d[r0:r0 + 128, :], in_=Ye[pu]), reads=[("Ye", pu, db) for db in range(4)], writes=[("yd", u)], dma=K(("ye", pu)))

    NST = CAP // 128
    cast_items(0)
    units = [(e_, st) for e_ in range(NE) for st in range(NST)]
    NU = len(units)
    assert NST == 2
    load_xg(units[0][0], units[0][1], 0)
    load_xg(units[1][0], units[1][1], 1)
    expert_unit(units[0][0], units[0][1], 0, "front")

    def back_and_next_front(u):
        a_ops, _ = P.capture(lambda: expert_unit(units[u][0], units[u][1], u, "b23"))
        b_ops = []
        if u + 1 < NU:
            def nf():
                if u + 2 < NU:
                    load_xg(units[u + 2][0], units[u + 2][1], u + 2)
                expert_unit(units[u + 1][0], units[u + 1][1], u + 1, "front")
            b_ops, _ = P.capture(nf)
        P.ops.extend(Prog.merge(a_ops, b_ops))

    for e_ in range(NE):
        u0 = 2 * e_

        def part1():
            expert_unit(e_, 0, u0, "b1")
            back_and_next_front(u0)
            expert_unit(e_, 1, u0 + 1, "b1")

        def cast_stream():
            if e_ + 1 < NE:
                cast_items(e_ + 1)
        a_ops, _ = P.capture(part1)
        b_ops, _ = P.capture(cast_stream)
        P.ops.extend(Prog.merge(a_ops, b_ops))
        back_and_next_front(u0 + 1)
    u = NU
    YD = [("yd", i) for i in range(u)]

    P.barrier(bar)
    RC.reset()
    y1b = [RC.alloc(DM * 2, BF16) for _ in range(2)]
    y2b = [RC.alloc(DM * 2, BF16) for _ in range(2)]
    xfb = [RC.alloc(DM * 4, F32) for _ in range(2)]

    def final_tile(j):
        rsl = slice(j * 128, (j + 1) * 128)
        fj = j % 2
        y1, y2, xf = y1b[fj], y2b[fj], xfb[fj]
        P.add("pool", lambda e: e.indirect_dma_start(out=y1[:, :], out_offset=None, in_=yb_d[:, :],
                                                     in_offset=bass.IndirectOffsetOnAxis(ap=pos1g[:, j:j + 1], axis=0),
                                                     bounds_check=NE * CAP - 1, oob_is_err=False),
              reads=YD + [("posG", "p1", j)] + mix_res, writes=[("y1", fj)], dma=K(("y1", fj)))
        P.add("pool", lambda e: e.indirect_dma_start(out=y2[:, :], out_offset=None, in_=yb_d[:, :],
                                                     in_offset=bass.IndirectOffsetOnAxis(ap=pos2g[:, j:j + 1], axis=0),
                                                     bounds_check=NE * CAP - 1, oob_is_err=False),
              reads=YD + [("posG", "p2", j)] + mix_res, writes=[("y2", fj)], dma=K(("y2", fj)))
        P.add("sp", lambda e: e.dma_start(out=xf, in_=x1_d[rsl, :]), reads=[("x1d", j)] + mix_res, writes=[("xf", fj)], dma=K(("xf", fj)))
        P.add("dve", lambda e: e.scalar_tensor_tensor(out=xf, in0=y1, scalar=cw1[:, j:j + 1], in1=xf, op0=ALU.mult, op1=ALU.add),
              reads=[("y1", fj), ("xf", fj), ("cw", "p1", j)], writes=[("xf", fj)])
        P.add("dve", lambda e: e.scalar_tensor_tensor(out=xf, in0=y2, scalar=cw2[:, j:j + 1], in1=xf, op0=ALU.mult, op1=ALU.add),
              reads=[("y2", fj), ("xf", fj), ("cw", "p2", j)], writes=[("xf", fj)])
        P.add("sp", lambda e: e.dma_start(out=out_d[rsl, :], in_=xf), reads=[("xf", fj)], writes=[("outd", j)], dma=K(("outd", fj)))

    for j in range(NTO):
        final_tile(j)
    g["final_reads"].extend([("outd", j) for j in range(NTO)])
```

```python
import numpy as np
import concourse.bass as bass
import concourse.mybir as mybir
from concourse.bass_utils import run_bass_kernel_spmd

F32 = mybir.dt.float32
BF16 = mybir.dt.bfloat16
U8 = mybir.dt.uint8
I32 = mybir.dt.int32
AF = mybir.ActivationFunctionType
ALU = mybir.AluOpType
AX = mybir.AxisListType


STRICT_WAR = True


class Prog:
    ENGS = ("pe", "act", "dve", "pool", "sp")

    def __init__(self, nc):
        self.nc = nc
        self.ops = []

    def add(self, eng, fn, reads=(), writes=(), dma=None, nophase=False):
        reads = tuple(reads)
        if not nophase:
            reads = reads + ("PHASE",)
        self.ops.append(dict(eng=eng, fn=fn, reads=reads, writes=tuple(writes),
                             dma=dma, deps=set(), sig=False))

    def capture(self, fn):
        old_ops = self.ops
        self.ops = []
        ret = fn()
        got = self.ops
        self.ops = old_ops
        return got, ret

    @staticmethod
    def merge(a, b):
        out, ia, ib = [], 0, 0
        while ia < len(a) or ib < len(b):
            if ib >= len(b) or (ia < len(a) and ia * len(b) <= ib * len(a)):
                out.append(a[ia]); ia += 1
            else:
                out.append(b[ib]); ib += 1
        return out

    def barrier(self, scratch):
        self.add("dve", lambda e: e.memset(scratch, 0.0), reads=(), writes=("PHASE", "bar_scratch"), nophase=True)

    def _analyse(self):
        last_w = {}
        readers = {}
        for i, op in enumerate(self.ops):
            deps = set()
            for r in op["reads"]:
                if r in last_w:
                    deps.add((last_w[r], "raw"))
            for w in op["writes"]:
                if w in last_w:
                    deps.add((last_w[w], "waw"))
                for rd in readers.get(w, ()):
                    deps.add((rd, "war"))
            real = set()
            for (d, kind) in deps:
                if d == i:
                    continue
                dop = self.ops[d]
                if dop["dma"] is None and dop["eng"] == op["eng"] and op["dma"] is None:
                    if op["eng"] == "pe" or (kind == "war" and not STRICT_WAR):
                        continue
                real.add(d)
            op["deps"] = real
            for d in real:
                self.ops[d]["sig"] = True
            for r in op["reads"]:
                readers.setdefault(r, []).append(i)
            for w in op["writes"]:
                last_w[w] = i
                readers[w] = []

    def emit(self, sems, dma_sems):
        self._analyse()
        nc = self.nc
        cnt = {e: 0 for e in self.ENGS}
        dcnt = {}
        for op in self.ops:
            if op["dma"] is not None:
                k = op["dma"]
                dcnt[k] = dcnt.get(k, 0) + 16
                op["tok"] = (dma_sems[k], dcnt[k])
            elif op["sig"]:
                cnt[op["eng"]] += 1
                op["tok"] = (sems[op["eng"]], cnt[op["eng"]])
        by_eng = {e: [op for op in self.ops if op["eng"] == e] for e in self.ENGS}
        ops = self.ops

        def run(eng_handle, lst):
            have = {}
            for op in lst:
                need = {}
                for d in op["deps"]:
                    s, v = ops[d]["tok"]
                    key = id(s)
                    if need.get(key, (None, 0))[1] < v:
                        need[key] = (s, v)
                for key, (s, v) in need.items():
                    if have.get(key, 0) < v:
                        eng_handle.wait_ge(s, v)
                        have[key] = v
                ins = op["fn"](eng_handle)
                if op["dma"] is not None:
                    ins.then_inc(op["tok"][0], 16)
                elif op["sig"]:
                    ins.then_inc(op["tok"][0], 1)

        with nc.Block() as block:
            @block.tensor
            def _(e):
                run(e, by_eng["pe"])

            @block.scalar
            def _(e):
                run(e, by_eng["act"])

            @block.vector
            def _(e):
                run(e, by_eng["dve"])

            @block.gpsimd
            def _(e):
                run(e, by_eng["pool"])

            @block.sync
            def _(e):
                run(e, by_eng["sp"])


class Region:
    def __init__(self, big, lo, hi):
        self.big, self.lo, self.hi, self.cur = big, lo, hi, lo

    def reset(self):
        self.cur = self.lo

    def alloc(self, nbytes, dtype, shape=None):
        a = (self.cur + 31) // 32 * 32
        assert a + nbytes <= self.hi, ("region overflow", a, nbytes, self.hi)
        self.cur = a + nbytes
        ap = self.big[:, a:a + nbytes]
        if dtype is not U8:
            ap = ap.bitcast(dtype)
        if shape is not None and len(shape) == 2:
            ap = ap.rearrange("p (a b) -> p a b", b=shape[1])
        elif shape is not None and len(shape) == 3:
            ap = ap.rearrange("p (a b c) -> p a b c", b=shape[1], c=shape[2])
        return ap


DM = 2048
NTO = 8
NTA = 16
KC = 16
C_DQ, C_DK, C_DV, C_GQ, C_GK, C_GV, C_GR, C_GA = 0, 1024, 2048, 3072, 3584, 4096, 5120, 6144
D_IN = 6160
NE = 32
FF = 512
CAP = 256
EPS = 1e-6
LAMBDA_INIT = 0.2
TWO_PI = 2.0 * np.pi
CW1 = 6.28125
CW2 = TWO_PI - CW1

CA_ID, CA_TRI, CA_TRS, CA_ONE, CA_FLG, CA_DM = 0, 128, 256, 384, 512, 640
NCA = 640 + 4 * 512
CB_GQ, CB_GK, CB_LAM, CB_GOUT, CB_FLAG, CB_BG, CB_GOG, CB_RB, CB_INVF, CB_PB = 0, 64, 128, 384, 385, 392, 904, 1160, 1196, 1204
NCB = 1240


def build_program(stage="full"):
    nc = bass.Bass("TRN2", target_bir_lowering=False)

    def din(name, shape, dtype=F32):
        return nc.dram_tensor(name, shape, dtype, kind="ExternalInput").ap()

    x_own = din("x_own", [1024, DM])
    x_oth = din("x_oth", [1024, DM])
    pos_d = din("pos", [128, NTA], I32)
    cstA_d = din("cstA", [128, NCA])
    cstB_d = din("cstB", [128, NCB])
    g1_d = din("g1rep", [128, DM])
    g2_d = din("g2rep", [128, DM])
    wg2_d = din("wg2", [16, 512])
    w_in = din("w_in", [DM, D_IN])
    w_out = din("w_out", [DM, DM])
    wr_d = din("wr", [DM, 36])
    w_gate = din("w_gate", [NE, DM, FF])
    w_up = din("w_up", [NE, DM, FF])
    w_down = din("w_down", [NE, FF, DM])
    out_d = nc.dram_tensor("out", [1024, DM], F32, kind="ExternalOutput").ap()
    if stage != "full":
        dbg_d = nc.dram_tensor("dbg", [128, 8192], F32, kind="ExternalOutput").ap()
        dbgb_d = nc.dram_tensor("dbgb", [128, 16384], BF16, kind="ExternalOutput").ap()
    else:
        dbg_d = dbgb_d = None
    xg_d = nc.dram_tensor("xg_scr", [NE * CAP, DM], BF16, kind="Internal").ap()
    yb_d = nc.dram_tensor("y_scr", [NE * CAP, DM], BF16, kind="Internal").ap()
    x1_d = nc.dram_tensor("x1_scr", [1024, DM], F32, kind="Internal").ap()
    STAGED = (0, 3, 5, 8, 11, 13, 16, 19, 21, 24, 27, 29)
    CONV_SET = [e_ for e_ in range(NE) if e_ not in STAGED]
    NCONV = len(CONV_SET)
    wcb_d = nc.dram_tensor("wcb_scr", [NCONV * 3, 128, 8192], BF16, kind="Internal").ap()

    w_in_v = w_in.rearrange("(kc p) c -> p kc c", p=128)
    w_out_v = w_out.rearrange("(kc p) c -> p kc c", p=128)
    wr_v = wr_d.rearrange("(kc p) c -> p kc c", p=128)

    dma_keys = []

    def K(k):
        if k not in dma_keys:
            dma_keys.append(k)
        return k

    import contextlib
    with contextlib.ExitStack() as es:
        big = es.enter_context(nc.sbuf_tensor("big", [128, 207 * 1024], U8))
        ps = [es.enter_context(nc.psum_tensor("ps%d" % i, [128, 512], F32)) for i in range(8)]
        psb = [p[:, :].bitcast(BF16) for p in ps]
        P = Prog(nc)
        RA = Region(big, 0, 64 * 1024)
        RB = Region(big, 64 * 1024, 96 * 1024)
        RC = Region(big, 96 * 1024, 128 * 1024)
        RD = Region(big, 128 * 1024, 190 * 1024)
        RE = Region(big, 190 * 1024, 207 * 1024)

        PSR = lambda b: ("ps", b)

        ident_bf = RE.alloc(256, BF16)
        ones_bf = RE.alloc(256, BF16)
        flag_bf = RE.alloc(256, BF16)
        trs_bf = RE.alloc(256, BF16)
        dmask_bf = RE.alloc(4 * 1024, BF16, [4, 512])
        tri_f = RE.alloc(512, F32)
        ones_f = RE.alloc(512, F32)
        cB = RE.alloc(NCB * 4, F32)
        wg2_f = RE.alloc(2048, F32)
        wr_bf = RE.alloc(KC * 36 * 2, BF16, [KC, 36])
        cos_t = RE.alloc(NTA * 8 * 4, F32, [NTA, 8])
        sin_t = RE.alloc(NTA * 8 * 4, F32, [NTA, 8])
        neglam = RE.alloc(4, F32)
        gout_s = RE.alloc(4, F32)
        gq_s = RE.alloc(256, F32)
        sm = RE.alloc(128 * 4, F32)
        flagcol = cB[:, CB_FLAG:CB_FLAG + 1]
        bar = RE.alloc(32, F32)

        hT = RA.alloc(KC * 2048 * 2, BF16, [KC, 2048])
        wbuf = [RB.alloc(KC * 512 * 2, BF16, [KC, 512]) for _ in range(2)]
        mixT = RC.alloc(KC * 1024 * 2, BF16, [KC, 1024])

        RD.reset()
        cA = RD.alloc(NCA * 4, F32)
        pos_i = RD.alloc(NTA * 4, I32)
        pos_f = RD.alloc(NTA * 4, F32)
        ang = RD.alloc(NTA * 8 * 4, F32, [NTA, 8])
        angc = RD.alloc(NTA * 8 * 4, F32, [NTA, 8])
        kf = RD.alloc(NTA * 8 * 4, F32, [NTA, 8])
        ki = RD.alloc(NTA * 8 * 4, I32, [NTA, 8])
        mk = RD.alloc(NTA * 8 * 4, F32, [NTA, 8])
        g1 = RD.alloc(DM * 4, F32)
        xt = [RD.alloc(DM * 4, F32) for _ in range(3)]
        hb = [RD.alloc(DM * 2, BF16) for _ in range(2)]
        sqj = RD.alloc(DM * 2, BF16)
        ss1 = RD.alloc(NTA * 4, F32)
        rs1 = RD.alloc(NTA * 4, F32)

        P.add("sp", lambda e: e.dma_start(out=cA, in_=cstA_d[:, :]), writes=["cA"], dma=K("cA"))
        P.add("sp", lambda e: e.dma_start(out=cB, in_=cstB_d[:, :]), writes=["cB"], dma=K("cB"))
        P.add("sp", lambda e: e.dma_start(out=pos_i, in_=pos_d[:, :]), writes=["pos_i"], dma=K("pos"))
        P.add("sp", lambda e: e.dma_start(out=wg2_f[0:16, :], in_=wg2_d[:, :]), writes=["wg2"], dma=K("wg2"))
        P.add("sp", lambda e: e.dma_start(out=g1, in_=g1_d[:, :]), writes=["g1"], dma=K("g1"))
        P.add("pool", lambda e: e.dma_start(out=wr_bf, in_=wr_v), writes=["wr"], dma=K("wr"))
        for (dst, c0, n, nm) in ((ident_bf, CA_ID, 128, "ident"), (ones_bf, CA_ONE, 128, "ones"), (flag_bf, CA_FLG, 128, "flagm"),
                                 (trs_bf, CA_TRS, 128, "trs"), (dmask_bf.rearrange("p a b -> p (a b)"), CA_DM, 2048, "dmask"),
                                 (tri_f, CA_TRI, 128, "tri_f"), (ones_f, CA_ONE, 128, "ones_f")):
            P.add("dve", (lambda dst, c0, n: lambda e: e.tensor_copy(out=dst, in_=cA[:, c0:c0 + n]))(dst, c0, n),
                  reads=["cA"], writes=[nm])

        P.add("dve", lambda e: e.tensor_copy(out=pos_f, in_=pos_i), reads=["pos_i"], writes=["pos_f"])
        P.add("dve", lambda e: e.tensor_tensor(out=ang, in0=pos_f.unsqueeze(2).to_broadcast([128, NTA, 8]),
                                               in1=cB[:, CB_INVF:CB_INVF + 8].unsqueeze(1).to_broadcast([128, NTA, 8]), op=ALU.mult),
              reads=["pos_f", "cB"], writes=["ang"])
        P.add("dve", lambda e: e.tensor_scalar_add(out=angc, in0=ang, scalar1=float(np.pi / 2)), reads=["ang"], writes=["angc"])

        def range_reduce(a, nm):
            P.add("dve", lambda e: e.tensor_scalar_mul(out=ki, in0=a, scalar1=float(1.0 / TWO_PI)), reads=[nm], writes=["ki"])
            P.add("dve", lambda e: e.tensor_copy(out=kf, in_=ki), reads=["ki"], writes=["kf"])
            P.add("dve", lambda e: e.scalar_tensor_tensor(out=a, in0=kf, scalar=-CW1, in1=a, op0=ALU.mult, op1=ALU.add),
                  reads=["kf", nm], writes=[nm])
            P.add("dve", lambda e: e.scalar_tensor_tensor(out=a, in0=kf, scalar=-CW2, in1=a, op0=ALU.mult, op1=ALU.add),
                  reads=["kf", nm], writes=[nm])
            P.add("dve", lambda e: e.tensor_single_scalar(out=mk, in_=a, scalar=float(np.pi), op=ALU.is_gt), reads=[nm], writes=["mk"])
            P.add("dve", lambda e: e.scalar_tensor_tensor(out=a, in0=mk, scalar=-TWO_PI, in1=a, op0=ALU.mult, op1=ALU.add),
                  reads=["mk", nm], writes=[nm])
            P.add("dve", lambda e: e.tensor_single_scalar(out=mk, in_=a, scalar=float(-np.pi), op=ALU.is_lt), reads=[nm], writes=["mk"])
            P.add("dve", lambda e: e.scalar_tensor_tensor(out=a, in0=mk, scalar=TWO_PI, in1=a, op0=ALU.mult, op1=ALU.add),
                  reads=["mk", nm], writes=[nm])
            P.add("dve", lambda e: e.tensor_scalar(out=a, in0=a, scalar1=3.1415925, scalar2=-3.1415925, op0=ALU.min, op1=ALU.max),
                  reads=[nm], writes=[nm])

        range_reduce(ang, "ang")
        range_reduce(angc, "angc")
        P.add("act", lambda e: e.activation(out=sin_t, in_=ang, func=AF.Sin), reads=["ang"], writes=["sin"])
        P.add("act", lambda e: e.activation(out=cos_t, in_=angc, func=AF.Sin), reads=["angc"], writes=["cos"])

        lv = cB[:, CB_LAM:CB_LAM + 256]
        P.add("dve", lambda e: e.tensor_tensor(out=sm[:, 0:64], in0=lv[:, 0:64], in1=lv[:, 64:128], op=ALU.mult), reads=["cB"], writes=["sm"])
        P.add("dve", lambda e: e.tensor_reduce(out=sm[:, 64:65], in_=sm[:, 0:64], axis=AX.X, op=ALU.add), reads=["sm"], writes=["sm1"])
        P.add("dve", lambda e: e.tensor_tensor(out=sm[:, 0:64], in0=lv[:, 128:192], in1=lv[:, 192:256], op=ALU.mult),
              reads=["cB", "sm1"], writes=["sm"])
        P.add("dve", lambda e: e.tensor_reduce(out=sm[:, 65:66], in_=sm[:, 0:64], axis=AX.X, op=ALU.add), reads=["sm"], writes=["sm2"])
        P.add("act", lambda e: e.activation(out=sm[:, 66:68], in_=sm[:, 64:66], func=AF.Exp), reads=["sm1", "sm2"], writes=["sm3"])
        P.add("dve", lambda e: e.tensor_tensor(out=neglam, in0=sm[:, 67:68], in1=sm[:, 66:67], op=ALU.subtract), reads=["sm3"], writes=["neglam"])
        P.add("dve", lambda e: e.tensor_scalar_add(out=neglam, in0=neglam, scalar1=-LAMBDA_INIT), reads=["neglam"], writes=["neglam"])
        P.add("dve", lambda e: e.tensor_scalar_mul(out=gout_s, in0=cB[:, CB_GOUT:CB_GOUT + 1], scalar1=1.0 - LAMBDA_INIT),
              reads=["cB"], writes=["gout_s"])
        P.add("dve", lambda e: e.tensor_scalar_mul(out=gq_s, in0=cB[:, CB_GQ:CB_GQ + 64], scalar1=0.125), reads=["cB"], writes=["gq_s"])
        gk_g = cB[:, CB_GK:CB_GK + 64]

        def x_tile_src(j):
            return (x_own if j < NTO else x_oth)[(j % NTO) * 128:(j % NTO + 1) * 128, :]

        for j in range(NTA):
            b = j % 3
            P.add("sp", (lambda j, b: lambda e: e.dma_start(out=xt[b], in_=x_tile_src(j)))(j, b), writes=[("xt", b)], dma=K(("xt", b)))
            P.add("act", (lambda j, b: lambda e: e.activation(out=sqj, in_=xt[b], func=AF.Square, accum_out=ss1[:, j:j + 1]))(j, b),
                  reads=[("xt", b)], writes=["sqj", ("ss1", j)])
            P.add("act", (lambda j: lambda e: e.activation(out=rs1[:, j:j + 1], in_=ss1[:, j:j + 1], func=AF.Ln, scale=1.0 / DM, bias=EPS))(j),
                  reads=[("ss1", j)], writes=[("rs1", j)])
            P.add("act", (lambda j: lambda e: e.activation(out=rs1[:, j:j + 1], in_=rs1[:, j:j + 1], func=AF.Exp, scale=-0.5))(j),
                  reads=[("rs1", j)], writes=[("rs1", j)])
            P.add("dve", (lambda j, b: lambda e: e.scalar_tensor_tensor(out=hb[j % 2], in0=xt[b], scalar=rs1[:, j:j + 1], in1=g1,
                                                                         op0=ALU.mult, op1=ALU.mult))(j, b),
                  reads=[("xt", b), ("rs1", j), "g1"], writes=[("hb", j % 2)])
            for half in range(2):
                bank = 2 + half
                for i in range(8):
                    kc = half * 8 + i
                    P.add("pe", (lambda j, kc, i, bank: lambda e: e.transpose(out=psb[bank][:, i * 128:(i + 1) * 128],
                                                                              in_=hb[j % 2][:, kc * 128:(kc + 1) * 128], identity=ident_bf))(j, kc, i, bank),
                          reads=[("hb", j % 2), "ident"], writes=[PSR(bank)])
                eng = "act" if half == 0 else "dve"
                if eng == "act":
                    P.add("act", (lambda j, half, bank: lambda e: e.activation(out=hT[:, half * 8:half * 8 + 8, j * 128:(j + 1) * 128],
                                                                               in_=psb[bank].rearrange("p (a b) -> p a b", b=128), func=AF.Copy))(j, half, bank),
                          reads=[], writes=[PSR(bank), ("hT", j)])
                else:
                    P.add("dve", (lambda j, half, bank: lambda e: e.tensor_copy(out=hT[:, half * 8:half * 8 + 8, j * 128:(j + 1) * 128],
                                                                                in_=psb[bank].rearrange("p (a b) -> p a b", b=128)))(j, half, bank),
                          reads=[], writes=[PSR(bank), ("hT", j)])

        final_reads = []
        conv_next = [0]

        def emit_conv(k):
            for _ in range(k):
                m = conv_next[0]
                if m >= NCONV * 3:
                    return
                conv_next[0] += 1
                e_ = CONV_SET[m // 3]
                kind = m % 3
                if kind == 0:
                    src = w_gate[e_].rearrange("(kc p) f -> p kc f", p=128)
                    dst = wcb_d[m].rearrange("p (a b) -> p a b", b=FF)
                elif kind == 1:
                    src = w_up[e_].rearrange("(kc p) f -> p kc f", p=128)
                    dst = wcb_d[m].rearrange("p (a b) -> p a b", b=FF)
                else:
                    src = w_down[e_].rearrange("(fc p) d -> p fc d", p=128)
                    dst = wcb_d[m].rearrange("p (a b) -> p a b", b=DM)
                P.add("pool", (lambda src, dst: lambda e: e.dma_start(out=dst, in_=src))(src, dst), writes=[("conv", m)], dma=K("conv"), nophase=True)

        ctx = dict(emit_conv=emit_conv, NCONV=NCONV, CONV_SET=CONV_SET, wcb_d=wcb_d, nc=nc, P=P, K=K, ps=ps, psb=psb, PSR=PSR, hT=hT, wbuf=wbuf, mixT=mixT, w_in_v=w_in_v,
                   RA=RA, RB=RB, RC=RC, RD=RD, RE=RE, ident_bf=ident_bf, ones_bf=ones_bf, flag_bf=flag_bf, trs_bf=trs_bf,
                   dmask_bf=dmask_bf, tri_f=tri_f, ones_f=ones_f, cB=cB, wg2_f=wg2_f, wr_bf=wr_bf, cos_t=cos_t, sin_t=sin_t,
                   bar=bar, neglam=neglam, gout_s=gout_s, gq_s=gq_s, gk_g=gk_g, flagcol=flagcol, sm=sm,
                   dbg_d=dbg_d, dbgb_d=dbgb_d, out_d=out_d, x_own=x_own, xg_d=xg_d, yb_d=yb_d, x1_d=x1_d, g2_d=g2_d,
                   w_out_v=w_out_v, w_gate=w_gate, w_up=w_up, w_down=w_down, stage=stage, final_reads=final_reads)

        if stage == "p1":
            RD.reset()
            d1 = RD.alloc(8192 * 4, F32)
            P.add("dve", lambda e: e.tensor_copy(out=d1[:, 0:4096].rearrange("p (a b) -> p a b", b=2048), in_=hT[:, 0:2, :]),
                  reads=[("hT", j) for j in range(NTA)], writes=["d1"])
            P.add("dve", lambda e: e.tensor_copy(out=d1[:, 4096:4096 + 128], in_=cos_t.rearrange("p a b -> p (a b)")), reads=["cos"], writes=["d1b"])
            P.add("dve", lambda e: e.tensor_copy(out=d1[:, 4224:4224 + 128], in_=sin_t.rearrange("p a b -> p (a b)")), reads=["sin"], writes=["d1c"])
            P.add("dve", lambda e: e.tensor_copy(out=d1[:, 4352:4353], in_=neglam), reads=["neglam"], writes=["d1d"])
            P.add("sp", lambda e: e.dma_start(out=dbg_d[:, :], in_=d1), reads=["d1", "d1b", "d1c", "d1d"], writes=["dbg"], dma=K("dbg"))
        else:
            mixer_stage(ctx)

        P.add("sp", lambda e: e.nop(), reads=["dbg"] + final_reads)
        sem_e = {k: es.enter_context(nc.semaphore("s_" + k)) for k in Prog.ENGS}
        sem_d = {k: es.enter_context(nc.semaphore("d%d" % i)) for i, k in enumerate(dma_keys)}
        P.emit(sem_e, sem_d)
    return nc


def _consts(flag):
    t = np.arange(128)
    cA = np.zeros((128, NCA), np.float32)
    cA[:, CA_ID:CA_ID + 128] = np.eye(128, dtype=np.float32)
    cA[:, CA_TRI:CA_TRI + 128] = (t[:, None] <= t[None, :])
    cA[:, CA_TRS:CA_TRS + 128] = (t[:, None] < t[None, :])
    cA[:, CA_ONE:CA_ONE + 128] = 1.0
    cA[:, CA_FLG:CA_FLG + 128] = flag
    for r in range(4):
        for c in range(4):
            blk = cA[:, CA_DM + r * 512 + c * 128: CA_DM + r * 512 + (c + 1) * 128]
            if c > r:
                blk[:] = 1.0
            elif c == r:
                blk[:] = (t[:, None] // 64 <= t[None, :] // 64)
    return cA


def prep_inputs(inputs, cores=range(8)):
    f = lambda k: np.ascontiguousarray(np.asarray(inputs[k]))
    x = f("x")
    pos = f("positions")
    rep = lambda v: np.ascontiguousarray(np.broadcast_to(np.asarray(v, np.float32).reshape(1, -1), (128, np.asarray(v).size)))
    cB0 = np.zeros((128, NCB), np.float32)
    cB0[:, CB_GQ:CB_GQ + 64] = rep(f("q_norm_g")[0])
    cB0[:, CB_GK:CB_GK + 64] = rep(f("k_norm_g")[0])
    cB0[:, CB_LAM:CB_LAM + 256] = rep(np.concatenate([f("lambda_q1")[0], f("lambda_k1")[0], f("lambda_q2")[0], f("lambda_k2")[0]]))
    cB0[:, CB_GOUT] = f("diff_out_norm_g")[0]
    cB0[:, CB_BG:CB_BG + 512] = rep(f("gla_b_gate")[0])
    cB0[:, CB_GOG:CB_GOG + 256] = rep(f("gla_out_norm_g")[0])
    cB0[:, CB_RB:CB_RB + 36] = rep(np.concatenate([f("b_router_group")[0], f("b_router_expert")[0].reshape(-1)]))
    cB0[:, CB_INVF:CB_INVF + 8] = rep(np.float32(500000.0) ** (-np.arange(0, 16, 2, dtype=np.float32) / np.float32(16)))
    cB0[:, CB_PB:CB_PB + 32] = rep(np.arange(32, dtype=np.float32) * CAP)
    wr = np.ascontiguousarray(np.concatenate([f("w_router_group")[0], f("w_router_expert")[0].transpose(1, 0, 2).reshape(DM, 32)], axis=1))
    shared = dict(
        g1rep=rep(f("norm1_g")[0]), g2rep=rep(f("norm2_g")[0]), wg2=f("gla_w_gate2")[0],
        w_in=f("w_in")[0], w_out=f("w_out")[0], wr=wr,
        w_gate=f("w_gate_expert")[0].reshape(NE, DM, FF), w_up=f("w_up_expert")[0].reshape(NE, DM, FF),
        w_down=f("w_down_expert")[0].reshape(NE, FF, DM))
    maps = []
    for c in cores:
        b, half = c // 2, c % 2
        cB = cB0.copy()
        cB[:, CB_FLAG] = float(half)
        po = pos[b, half * 1024:(half + 1) * 1024].reshape(8, 128)
        pt = pos[b, 0:1024].reshape(8, 128)
        m = dict(shared)
        m.update(x_own=np.ascontiguousarray(x[b, half * 1024:(half + 1) * 1024]), x_oth=np.ascontiguousarray(x[b, 0:1024]),
                 pos=np.ascontiguousarray(np.concatenate([po, pt], 0).T.astype(np.int32)),
                 cstA=_consts(float(half)), cstB=cB)
        maps.append(m)
    return maps


_NC_CACHE = {}


def kernel(**inputs):
    if "full" not in _NC_CACHE:
        _NC_CACHE["full"] = build_program("full")
    nc = _NC_CACHE["full"]
    maps = prep_inputs(inputs)
    res = run_bass_kernel_spmd(nc, maps, core_ids=list(range(8)))
    out = np.zeros((4, 2048, DM), np.float32)
    for c in range(8):
        b, half = c // 2, c % 2
        out[b, half * 1024:(half + 1) * 1024] = res.results[c]["out"]
    return out


def mixer_stage(ctx):
    g = dict(ctx)
    nc, P, K, ps, psb, PSR = (g[k] for k in ("nc", "P", "K", "ps", "psb", "PSR"))
    hT, wbuf, mixT, w_in_v, RD, cB = (g[k] for k in ("hT", "wbuf", "mixT", "w_in_v", "RD", "cB"))
    ident_bf, ones_bf, flag_bf, dmask_bf = (g[k] for k in ("ident_bf", "ones_bf", "flag_bf", "dmask_bf"))
    cos_t, sin_t, neglam, gout_s, gq_s, gk_g, flagcol = (g[k] for k in ("cos_t", "sin_t", "neglam", "gout_s", "gq_s", "gk_g", "flagcol"))
    stage = g["stage"]
    OWN = list(range(NTO))
    ALL = list(range(NTA))
    wslot = [0]
    mmbank = [0]
    mmbanks = [[0, 1]]

    emit_conv = g["emit_conv"]

    def load_w(c0, n):
        s = wslot[0]
        wslot[0] ^= 1
        P.add("pool", lambda e: e.dma_start(out=wbuf[s][:, :, 0:n], in_=w_in_v[:, :, c0:c0 + n]), writes=[("wb", s)], dma=K(("wb", s)), nophase=True)
        emit_conv(2)
        return s

    def proj_tile(s, n, j):
        bl = mmbanks[0]
        b = bl[mmbank[0] % len(bl)]
        mmbank[0] += 1
        for kc in range(KC):
            P.add("pe", (lambda kc: lambda e: e.matmul(ps[b][:, 0:n], lhsT=hT[:, kc, j * 128:(j + 1) * 128], rhs=wbuf[s][:, kc, 0:n],
                                                       start=(kc == 0), stop=(kc == KC - 1)))(kc),
                  reads=[("hT", j), ("wb", s)], writes=[PSR(b)])
        return b

    bar = g["bar"]
    P.barrier(bar)
    RD.reset()
    qT = RD.alloc(4 * 1024 * 2, BF16, [4, 1024])
    kT = RD.alloc(4 * 2048 * 2, BF16, [4, 2048])
    vv = RD.alloc(NTA * 512 * 2, BF16, [NTA, 512])
    shared_lo = RD.cur
    CT = []
    NCT = 3
    for _ in range(NCT):
        CT.append(dict(sqt=RD.alloc(2048, F32), qn=RD.alloc(2048, F32, [8, 64]), qb=RD.alloc(1024, BF16, [8, 64]), ssq=RD.alloc(32, F32),
                       rsq=RD.alloc(32, F32), rx=RD.alloc(8 * 16 * 4, F32, [8, 16]), rt=[RD.alloc(8 * 8 * 4, F32, [8, 8]) for _ in range(4)]))
    RD.cur = shared_lo
    Pm = [[RD.alloc(1024, BF16) for _ in range(2)] for _ in range(2)]
    rl = [RD.alloc(2048, F32) for _ in range(2)]
    tt = [RD.alloc(2048, F32) for _ in range(2)]
    ot = RD.alloc(2048, F32)
    osq = RD.alloc(1024, BF16)
    rstd_o = rl[0]
    ccnt = [0]

    xg_d = g["xg_d"]
    zsrc = vv[:, 0:4, :].rearrange("p a b -> p (a b)")
    P.add("dve", lambda e: e.memset(zsrc, 0.0), reads=[], writes=[("v", jj) for jj in range(4)])
    for u_ in range(NE * CAP // 128):
        P.add("sp", (lambda u_: lambda e: e.dma_start(out=xg_d[u_ * 128:(u_ + 1) * 128, :], in_=zsrc))(u_), reads=[("v", jj) for jj in range(4)],
              writes=[("xgd", u_)], dma=K("xgz"), nophase=True)

    def qk_consumer(j, b, is_q, dstT):
        gv = gq_s if is_q else gk_g
        nm = "q" if is_q else "k"
        cp = ccnt[0] % NCT
        ccnt[0] += 1
        c = CT[cp]
        sqt, qn, qb, ssq, rsq, rx, rt = c["sqt"], c["qn"], c["qb"], c["ssq"], c["rsq"], c["rx"], c["rt"]
        R = lambda x: (x, cp)
        tb = 2 + cp % 2
        psv = ps[b][:, 0:512]

        def stA():
            P.add("act", lambda e: e.activation(out=sqt, in_=psv, func=AF.Square), reads=[], writes=[PSR(b), R("sqt")])
            P.add("dve", lambda e: e.tensor_reduce(out=ssq, in_=sqt.rearrange("p (a b) -> p a b", b=64), axis=AX.X, op=ALU.add),
                  reads=[R("sqt")], writes=[R("ssq")])
            P.add("act", lambda e: e.activation(out=rsq, in_=ssq, func=AF.Ln, scale=1.0 / 64, bias=EPS), reads=[R("ssq")], writes=[R("rsq")])
            P.add("act", lambda e: e.activation(out=rsq, in_=rsq, func=AF.Exp, scale=-0.5), reads=[R("rsq")], writes=[R("rsq")])
            P.add("dve", lambda e: e.tensor_tensor(out=qn, in0=psv.rearrange("p (a b) -> p a b", b=64),
                                                   in1=rsq.unsqueeze(2).to_broadcast([128, 8, 64]), op=ALU.mult),
                  reads=[R("rsq")], writes=[PSR(b), R("qn")])

        def stB():
            P.add("pool", lambda e: e.tensor_tensor(out=qb[:, :, 16:64], in0=qn[:, :, 16:64], in1=gv[:, 16:64].unsqueeze(1).to_broadcast([128, 8, 48]), op=ALU.mult),
                  reads=[R("qn"), "gq_s", "cB"], writes=[R("qbhi")])
            P.add("dve", lambda e: e.tensor_tensor(out=rx, in0=qn[:, :, 0:16], in1=gv[:, 0:16].unsqueeze(1).to_broadcast([128, 8, 16]), op=ALU.mult),
                  reads=[R("qn"), "gq_s", "cB"], writes=[R("rx")])
            cj = cos_t[:, j, :].unsqueeze(1).to_broadcast([128, 8, 8])
            sj = sin_t[:, j, :].unsqueeze(1).to_broadcast([128, 8, 8])
            x1, x2 = rx[:, :, 0:8], rx[:, :, 8:16]
            P.add("dve", lambda e: e.tensor_tensor(out=rt[0], in0=x1, in1=cj, op=ALU.mult), reads=[R("rx"), "cos"], writes=[R("rt0")])
            P.add("dve", lambda e: e.tensor_tensor(out=rt[1], in0=x2, in1=sj, op=ALU.mult), reads=[R("rx"), "sin"], writes=[R("rt1")])
            P.add("dve", lambda e: e.tensor_tensor(out=rt[2], in0=x2, in1=cj, op=ALU.mult), reads=[R("rx"), "cos"], writes=[R("rt2")])
            P.add("dve", lambda e: e.tensor_tensor(out=rt[3], in0=x1, in1=sj, op=ALU.mult), reads=[R("rx"), "sin"], writes=[R("rt3")])
            P.add("dve", lambda e: e.tensor_tensor(out=qb[:, :, 0:8], in0=rt[0], in1=rt[1], op=ALU.subtract), reads=[R("rt0"), R("rt1")], writes=[R("qblo1")])
            P.add("dve", lambda e: e.tensor_tensor(out=qb[:, :, 8:16], in0=rt[2], in1=rt[3], op=ALU.add), reads=[R("rt2"), R("rt3")], writes=[R("qblo2")])

        def stC():
            for h in range(4):
                P.add("pe", (lambda h: lambda e: e.transpose(out=psb[tb][:, h * 128:(h + 1) * 128],
                                                             in_=qb[:, 2 * h:2 * h + 2, :].rearrange("p a b -> p (a b)"), identity=ident_bf))(h),
                      reads=[R("qbhi"), R("qblo1"), R("qblo2"), "ident"], writes=[PSR(tb)])
            P.add("act", lambda e: e.activation(out=dstT[:, :, j * 128:(j + 1) * 128], in_=psb[tb][:, 0:512].rearrange("p (a b) -> p a b", b=128),
                                                func=AF.Copy), reads=[], writes=[PSR(tb), (nm + "T", j)])
        return [stA, stB, stC]

    def v_consumer(j, b):
        psv = ps[b][:, 0:512]
        if j < NTO:
            P.add("act", lambda e: e.activation(out=vv[:, j, :], in_=psv, func=AF.Copy), reads=[], writes=[PSR(b), ("v", j)])
        else:
            P.add("dve", lambda e: e.tensor_scalar(out=vv[:, j, :], in0=psv, scalar1=flagcol, scalar2=None, op0=ALU.mult),
                  reads=["cB"], writes=[PSR(b), ("v", j)])

    O1, O2, L1, L2 = 6, 7, 2, 3
    SB = ((4, 5), (0, 1))

    def att_qk(hl, gq_, i, kt):
        S1, S2 = SB[i % 2]
        c0 = (kt - 4 * gq_) * 128 if (kt < NTO and kt >= 4 * gq_) else 0
        qs = slice(gq_ * 512 + c0, (gq_ + 1) * 512)
        ksl = slice(kt * 128, (kt + 1) * 128)
        qreads = [("qT", 4 * gq_ + c) for c in range(4)]
        P.add("pe", lambda e: e.matmul(ps[S1][:, c0:512], lhsT=kT[0:64, hl, ksl], rhs=qT[0:64, hl, qs], start=True, stop=True),
              reads=[("kT", kt)] + qreads, writes=[PSR(S1)])
        P.add("pe", lambda e: e.matmul(ps[S2][:, c0:512], lhsT=kT[64:128, hl, ksl], rhs=qT[64:128, hl, qs], start=True, stop=True),
              reads=[("kT", kt)] + qreads, writes=[PSR(S2)])

    def att_pv(hl, gq_, i, kt, nk):
        S1, S2 = SB[i % 2]
        pb = i % 2
        diag = (kt < NTO and kt >= 4 * gq_)
        c0 = (kt - 4 * gq_) * 128 if diag else 0
        P.add("act", lambda e: e.activation(out=Pm[0][pb][:, c0:512], in_=ps[S1][:, c0:512], func=AF.Exp), reads=[], writes=[PSR(S1), ("P0", pb)])
        P.add("act", lambda e: e.activation(out=Pm[1][pb][:, c0:512], in_=ps[S2][:, c0:512], func=AF.Exp), reads=[], writes=[PSR(S2), ("P1", pb)])
        if diag:
            P.add("dve", lambda e: e.tensor_tensor(out=Pm[0][pb][:, c0:c0 + 128], in0=Pm[0][pb][:, c0:c0 + 128], in1=dmask_bf[:, 0, 0:128], op=ALU.mult),
                  reads=["dmask", ("P0", pb)], writes=[("P0", pb)])
            P.add("dve", lambda e: e.tensor_tensor(out=Pm[1][pb][:, c0:c0 + 128], in0=Pm[1][pb][:, c0:c0 + 128], in1=dmask_bf[:, 0, 0:128], op=ALU.mult),
                  reads=["dmask", ("P1", pb)], writes=[("P1", pb)])
        lw = flag_bf if kt >= NTO else ones_bf
        first, last = (i == 0), (i == nk - 1)
        vsl = vv[:, kt, hl * 128:(hl + 1) * 128]
        P.add("pe", lambda e: e.matmul(ps[O1][:, c0:512], lhsT=vsl, rhs=Pm[0][pb][:, c0:512], start=first, stop=last), reads=[("v", kt), ("P0", pb)], writes=[PSR(O1)])
        P.add("pe", lambda e: e.matmul(ps[L1][:, c0:512], lhsT=lw, rhs=Pm[0][pb][:, c0:512], start=first, stop=last), reads=["ones", "flagm", ("P0", pb)], writes=[PSR(L1)])
        P.add("pe", lambda e: e.matmul(ps[O2][:, c0:512], lhsT=vsl, rhs=Pm[1][pb][:, c0:512], start=first, stop=last), reads=[("v", kt), ("P1", pb)], writes=[PSR(O2)])
        P.add("pe", lambda e: e.matmul(ps[L2][:, c0:512], lhsT=lw, rhs=Pm[1][pb][:, c0:512], start=first, stop=last), reads=["ones", "flagm", ("P1", pb)], writes=[PSR(L2)])

    def att_epilogue(hglob, gq_):
        qs = slice(gq_ * 512, (gq_ + 1) * 512)
        P.add("dve", lambda e: e.reciprocal(out=rl[0], in_=ps[L1][:, :]), reads=[], writes=[PSR(L1), ("rl", 0)])
        P.add("dve", lambda e: e.tensor_tensor(out=tt[0], in0=ps[O1][:, :], in1=rl[0], op=ALU.mult), reads=[("rl", 0)], writes=[PSR(O1), ("tt", 0)])
        P.add("dve", lambda e: e.reciprocal(out=rl[1], in_=ps[L2][:, :]), reads=[], writes=[PSR(L2), ("rl", 1)])
        P.add("dve", lambda e: e.tensor_tensor(out=tt[1], in0=ps[O2][:, :], in1=rl[1], op=ALU.mult), reads=[("rl", 1)], writes=[PSR(O2), ("tt", 1)])
        P.add("dve", lambda e: e.scalar_tensor_tensor(out=ot, in0=tt[1], scalar=neglam, in1=tt[0], op0=ALU.mult, op1=ALU.add),
              reads=[("tt", 0), ("tt", 1), "neglam"], writes=["ot"])
        P.add("act", lambda e: e.activation(out=osq, in_=ot, func=AF.Square), reads=["ot"], writes=["osq"])
        P.add("pe", lambda e: e.matmul(ps[L1][:, :], lhsT=ones_bf, rhs=osq, start=True, stop=True), reads=["ones", "osq"], writes=[PSR(L1)])
        P.add("act", lambda e: e.activation(out=rstd_o, in_=ps[L1][:, :], func=AF.Ln, scale=1.0 / 128, bias=EPS), reads=[], writes=[PSR(L1), ("rl", 0)])
        P.add("act", lambda e: e.activation(out=rstd_o, in_=rstd_o, func=AF.Exp, scale=-0.5), reads=[("rl", 0)], writes=[("rl", 0)])
        P.add("dve", lambda e: e.scalar_tensor_tensor(out=mixT[:, hglob, qs], in0=ot, scalar=gout_s, in1=rstd_o, op0=ALU.mult, op1=ALU.mult),
              reads=["ot", "gout_s", ("rl", 0)], writes=[("mixT", hglob, gq_)])

    def attention(hg):
        emit_conv(7)
        for hl in range(4):
            for gq_ in range(2):
                kts = list(range(NTO, NTA)) + list(range(0, 4 * gq_ + 4))
                att_qk(hl, gq_, 0, kts[0])
                for i, kt in enumerate(kts):
                    if i + 1 < len(kts):
                        att_qk(hl, gq_, i + 1, kts[i + 1])
                    att_pv(hl, gq_, i, kt, len(kts))
                att_epilogue(hg * 4 + hl, gq_)

    blocks = []
    for hg in range(2):
        blocks.append(("q", C_DQ + hg * 512, 512, OWN, hg))
        blocks.append(("k", C_DK + hg * 512, 512, ALL, hg))
        blocks.append(("v", C_DV + hg * 512, 512, ALL, hg))
    slots = [None] * len(blocks)
    mmbanks[0] = [0, 1, 6, 7]
    slots[0] = load_w(blocks[0][1], blocks[0][2])
    for bi, (kind, c0, n, tiles, hg) in enumerate(blocks):
        if bi + 1 < len(blocks):
            slots[bi + 1] = load_w(blocks[bi + 1][1], blocks[bi + 1][2])
        LA = 3
        nst = 1 if kind == "v" else 3
        pend = [proj_tile(slots[bi], n, tiles[t]) for t in range(min(LA, len(tiles)))]
        staged = {}
        for it in range(len(tiles) + nst - 1):
            if it < len(tiles):
                j = tiles[it]
                b = pend.pop(0)
                if it + LA < len(tiles):
                    pend.append(proj_tile(slots[bi], n, tiles[it + LA]))
                if kind == "q":
                    staged[it] = qk_consumer(j, b, True, qT)
                elif kind == "k":
                    staged[it] = qk_consumer(j, b, False, kT)
                else:
                    staged[it] = [(lambda j, b: lambda: v_consumer(j, b))(j, b)]
            for sidx in range(nst):
                t = it - sidx
                if 0 <= t < len(tiles):
                    staged[t][sidx]()
        if kind == "v" and stage == "qkv":
            P.add("sp", lambda e: e.dma_start(out=g["dbgb_d"][:, 0:4096], in_=qT.rearrange("p a b -> p (a b)")),
                  reads=[("qT", j) for j in range(NTO)], writes=["dbg0"], dma=K("dbg0"))
            P.add("sp", lambda e: e.dma_start(out=g["dbgb_d"][:, 4096:12288], in_=kT.rearrange("p a b -> p (a b)")),
                  reads=[("kT", j) for j in range(NTA)], writes=["dbg1"], dma=K("dbg1"))
            P.add("sp", lambda e: e.dma_start(out=g["dbgb_d"][:, 12288:16384], in_=vv[:, 8:16, :].rearrange("p a b -> p (a b)")),
                  reads=[("v", j) for j in range(NTA)], writes=["dbg"], dma=K("dbg"))
            return
        if kind == "v":
            P.barrier(bar)
            attention(hg)
            P.barrier(bar)
            if stage == "attn0":
                break

    if stage in ("attn0", "attn"):
        P.add("sp", lambda e: e.dma_start(out=g["dbgb_d"][:, :], in_=mixT.rearrange("p a b -> p (a b)")),
              reads=[("mixT", h, q) for h in range(8) for q in range(2)], writes=["dbg"], dma=K("dbg"))
        return

    tri_f, ones_f, wg2_f = g["tri_f"], g["ones_f"], g["wg2_f"]
    mmbanks[0] = [0, 1]
    P.barrier(bar)
    RD.reset()
    gaT = RD.alloc(2048 * 4, F32)
    gkb = RD.alloc(NTA * 256 * 2, BF16, [NTA, 256])
    gvb = RD.alloc(NTA * 512 * 2, BF16, [NTA, 512])
    gqb = RD.alloc(NTO * 256 * 2, BF16, [NTO, 256])
    grb = RD.alloc(NTO * 512 * 2, BF16, [NTO, 512])
    T = {}
    for nm in ("pre", "ee", "nla", "totS", "dlt", "E1", "E2", "E3"):
        T[nm] = [RD.alloc(1024, F32)] * 2
    for nm in ("qin", "kin"):
        T[nm] = [RD.alloc(512, BF16)] * 2
    T["kdec"] = [RD.alloc(512, BF16) for _ in range(2)]
    qkT = [RD.alloc(4 * 128 * 2, BF16, [4, 128]) for _ in range(2)]
    decb = [RD.alloc(16, F32) for _ in range(2)]
    attm = RD.alloc(256, BF16)
    S_f = RD.alloc(2 * 256 * 4, F32, [2, 256])
    S_bf = RD.alloc(2 * 256 * 2, BF16, [2, 256])
    on_ = RD.alloc(1024, F32)
    eg = RD.alloc(1024, F32)
    gob = RD.alloc(512, BF16)
    ssg = RD.alloc(4, F32)
    rsg = RD.alloc(4, F32)
    bg = cB[:, CB_BG:CB_BG + 512]
    gog = cB[:, CB_GOG:CB_GOG + 256]

    def ga_block(s):
        for tg in range(4):
            b = mmbank[0] % 2
            mmbank[0] += 1
            for kc in range(KC):
                P.add("pe", (lambda kc, tg, b: lambda e: e.matmul(ps[b][0:16, :], lhsT=wbuf[s][:, kc, 0:16], rhs=hT[:, kc, tg * 512:(tg + 1) * 512],
                                                                  start=(kc == 0), stop=(kc == KC - 1)))(kc, tg, b),
                      reads=[("hT", 4 * tg + c) for c in range(4)] + [("wb", s)], writes=[PSR(b)])
            P.add("act", (lambda tg, b: lambda e: e.activation(out=gaT[0:16, tg * 512:(tg + 1) * 512], in_=ps[b][0:16, :], func=AF.Copy))(tg, b),
                  reads=[], writes=[PSR(b), ("gaT", tg)])

    def copy_consumer(dst, nm, n):
        def f(j, b):
            if j % 2 == 0:
                P.add("act", lambda e: e.activation(out=dst[:, j, :], in_=ps[b][:, 0:n], func=AF.Copy), reads=[], writes=[PSR(b), (nm, j)])
            else:
                P.add("dve", lambda e: e.tensor_copy(out=dst[:, j, :], in_=ps[b][:, 0:n]), reads=[], writes=[PSR(b), (nm, j)])
        return f

    gcnt = [0]

    def gla_tile(hp, j, own):
        pj = 0
        pq = gcnt[0] % 2
        gcnt[0] += 1
        tsl = slice(j * 128, (j + 1) * 128)
        c0 = hp * 256
        pre, ee, nla, totS, dlt = T["pre"][pj], T["ee"][pj], T["nla"][pj], T["totS"][pj], T["dlt"][pj]
        E1, E2, E3, kdec, qin, kin = T["E1"][pj], T["E2"][pj], T["E3"][pj], T["kdec"][pq], T["qin"][pj], T["kin"][pj]
        dec, qk = decb[pq], qkT[pq]
        R = lambda nm: (nm, pq) if nm in ("kdec", "qk", "dec") else (nm, pj)
        P.add("pe", lambda e: e.matmul(ps[0][:, 0:256], lhsT=gaT[0:16, tsl], rhs=wg2_f[0:16, c0:c0 + 256], start=True, stop=True),
              reads=[("gaT", j // 4), "wg2"], writes=[PSR(0)])
        P.add("dve", lambda e: e.tensor_tensor(out=pre, in0=ps[0][:, 0:256], in1=bg[:, c0:c0 + 256], op=ALU.add), reads=["cB"], writes=[PSR(0), R("pre")])
        P.add("act", lambda e: e.activation(out=ee, in_=pre, func=AF.Exp, scale=-1.0), reads=[R("pre")], writes=[R("ee")])
        P.add("act", lambda e: e.activation(out=nla, in_=ee, func=AF.Ln, bias=1.0), reads=[R("ee")], writes=[R("nla")])
        P.add("pe", lambda e: e.matmul(ps[0][:, 0:256], lhsT=tri_f, rhs=nla, start=True, stop=True), reads=["tri_f", R("nla")], writes=[PSR(0)])
        P.add("pe", lambda e: e.matmul(ps[1][:, 0:256], lhsT=ones_f, rhs=nla, start=True, stop=True), reads=["ones_f", R("nla")], writes=[PSR(1)])
        for hh in range(2):
            P.add("pe", (lambda hh: lambda e: e.matmul(ps[2][:, 2 * hh:2 * hh + 2], lhsT=nla[:, hh * 128:(hh + 1) * 128], rhs=ones_f[:, 0:2],
                                                       start=True, stop=True))(hh), reads=["ones_f", R("nla")], writes=[PSR(2)])
        P.add("act", lambda e: e.activation(out=dec, in_=ps[2][:, 0:4], func=AF.Exp, scale=-1.0 / 16), reads=[], writes=[PSR(2), R("dec")])
        P.add("act", lambda e: e.activation(out=totS, in_=ps[1][:, 0:256], func=AF.Copy), reads=[], writes=[PSR(1), R("totS")])
        P.add("dve", lambda e: e.tensor_tensor(out=dlt, in0=ps[0][:, 0:256], in1=totS, op=ALU.subtract), reads=[R("totS")], writes=[PSR(0), R("dlt")])
        P.add("act", lambda e: e.activation(out=E3, in_=dlt, func=AF.Exp, scale=1.0 / 16), reads=[R("dlt")], writes=[R("E3")])
        P.add("dve", lambda e: e.tensor_tensor(out=kdec, in0=gkb[:, j, :], in1=E3, op=ALU.mult), reads=[("gk", j), R("E3")], writes=[R("kdec")])
        if own:
            P.add("act", lambda e: e.activation(out=E1, in_=ps[0][:, 0:256], func=AF.Exp, scale=-1.0 / 16), reads=[], writes=[PSR(0), R("E1")])
            P.add("act", lambda e: e.activation(out=E2, in_=ps[0][:, 0:256], func=AF.Exp, scale=1.0 / 16), reads=[], writes=[PSR(0), R("E2")])
            P.add("dve", lambda e: e.scalar_tensor_tensor(out=qin, in0=gqb[:, j, :], scalar=float(128 ** -0.5), in1=E1, op0=ALU.mult, op1=ALU.mult),
                  reads=[("gq", j), R("E1")], writes=[R("qin")])
            P.add("dve", lambda e: e.tensor_tensor(out=kin, in0=gkb[:, j, :], in1=E2, op=ALU.mult), reads=[("gk", j), R("E2")], writes=[R("kin")])
            for hh in range(2):
                P.add("pe", (lambda hh: lambda e: e.transpose(out=psb[3][:, hh * 128:(hh + 1) * 128], in_=qin[:, hh * 128:(hh + 1) * 128],
                                                              identity=ident_bf))(hh), reads=[R("qin"), "ident"], writes=[PSR(3)])
            for hh in range(2):
                P.add("pe", (lambda hh: lambda e: e.transpose(out=psb[3][:, 256 + hh * 128:256 + (hh + 1) * 128], in_=kin[:, hh * 128:(hh + 1) * 128],
                                                              identity=ident_bf))(hh), reads=[R("kin"), "ident"], writes=[PSR(3)])
            P.add("act", lambda e: e.activation(out=qk, in_=psb[3][:, 0:512].rearrange("p (a b) -> p a b", b=128), func=AF.Copy),
                  reads=[], writes=[PSR(3), R("qk")])
        def heads():
            for hh in range(2):
                gla_head(hp, j, own, hh, kdec, qk, dec, pq)
        return heads

    def gla_head(hp, j, own, hh, kdec, qk, dec, pq):
        tsl = slice(j * 128, (j + 1) * 128)
        R = lambda nm: (nm, pq)
        vsl = gvb[:, j, hh * 256:(hh + 1) * 256]
        if own:
            P.add("pe", lambda e: e.matmul(ps[4][:, 0:128], lhsT=qk[:, 2 + hh, :], rhs=qk[:, hh, :], start=True, stop=True), reads=[R("qk")], writes=[PSR(4)])
            P.add("dve", lambda e: e.tensor_tensor(out=attm, in0=ps[4][:, 0:128], in1=tri_f, op=ALU.mult), reads=["tri_f"], writes=[PSR(4), "attm"])
            P.add("pe", lambda e: e.matmul(ps[5][:, 0:256], lhsT=attm, rhs=vsl, start=True, stop=False), reads=["attm", ("gv", j)], writes=[PSR(5)])
            P.add("pe", lambda e: e.matmul(ps[5][:, 0:256], lhsT=qk[:, hh, :], rhs=S_bf[:, hh, :], start=False, stop=True), reads=[R("qk"), ("S_bf", hh)], writes=[PSR(5)])
            P.add("act", lambda e: e.activation(out=eg, in_=ps[5][:, 0:256], func=AF.Square, accum_out=ssg), reads=[], writes=[PSR(5), "eg", "ssg"])
            P.add("act", lambda e: e.activation(out=rsg, in_=ssg, func=AF.Ln, scale=1.0 / 256, bias=EPS), reads=["ssg"], writes=["rsg"])
            P.add("act", lambda e: e.activation(out=rsg, in_=rsg, func=AF.Exp, scale=-0.5), reads=["rsg"], writes=["rsg"])
            P.add("dve", lambda e: e.scalar_tensor_tensor(out=on_, in0=ps[5][:, 0:256], scalar=rsg, in1=gog, op0=ALU.mult, op1=ALU.mult),
                  reads=["rsg", "cB"], writes=[PSR(5), "on"])
            gsl = grb[:, j, hh * 256:(hh + 1) * 256]
            P.add("act", lambda e: e.activation(out=eg, in_=gsl, func=AF.Exp, scale=-1.0), reads=[("gr", j)], writes=["eg"])
            P.add("dve", lambda e: e.tensor_scalar_add(out=eg, in0=eg, scalar1=1.0), reads=["eg"], writes=["eg"])
            P.add("dve", lambda e: e.reciprocal(out=eg, in_=eg), reads=["eg"], writes=["eg"])
            P.add("dve", lambda e: e.tensor_tensor(out=on_, in0=on_, in1=eg, op=ALU.mult), reads=["on", "eg"], writes=["on"])
            P.add("dve", lambda e: e.tensor_tensor(out=gob, in0=on_, in1=gsl, op=ALU.mult), reads=["on", ("gr", j)], writes=["gob"])
            for c in range(2):
                P.add("pe", (lambda c: lambda e: e.transpose(out=psb[6][:, c * 128:(c + 1) * 128], in_=gob[:, c * 128:(c + 1) * 128], identity=ident_bf))(c),
                      reads=["gob", "ident"], writes=[PSR(6)])
            kc0 = 8 + 2 * (2 * hp + hh)
            P.add("act", lambda e: e.activation(out=mixT[:, kc0:kc0 + 2, tsl], in_=psb[6][:, 0:256].rearrange("p (a b) -> p a b", b=128), func=AF.Copy),
                  reads=[], writes=[PSR(6), ("mixTg", kc0, j)])
        P.add("pe", lambda e: e.matmul(ps[7][:, 0:256], lhsT=kdec[:, hh * 128:(hh + 1) * 128], rhs=vsl, start=True, stop=True),
              reads=[R("kdec"), ("gv", j)], writes=[PSR(7)])
        P.add("dve", lambda e: e.scalar_tensor_tensor(out=S_f[:, hh, :], in0=S_f[:, hh, :], scalar=dec[:, 2 * hh:2 * hh + 1], in1=ps[7][:, 0:256],
                                                      op0=ALU.mult, op1=ALU.add), reads=[R("dec"), ("S_f", hh)], writes=[PSR(7), ("S_f", hh)])
        P.add("act", lambda e: e.activation(out=S_bf[:, hh, :], in_=S_f[:, hh, :], func=AF.Copy), reads=[("S_f", hh)], writes=[("S_bf", hh)])

    def gla_scan(hp):
        P.add("dve", lambda e: e.memset(S_f.rearrange("p a b -> p (a b)"), 0.0), reads=[], writes=[("S_f", 0), ("S_f", 1)])
        P.add("dve", lambda e: e.memset(S_bf.rearrange("p a b -> p (a b)"), 0.0), reads=[], writes=[("S_bf", 0), ("S_bf", 1)])
        order = [(j, False) for j in range(NTO, NTA)] + [(j, True) for j in range(NTO)]
        emit_conv(7)

        def capture(fn):
            old_ops = P.ops
            P.ops = []
            ret = fn()
            got = P.ops
            P.ops = old_ops
            return got, ret

        def merge(a, b):
            out, ia, ib = [], 0, 0
            while ia < len(a) or ib < len(b):
                if ib >= len(b) or (ia < len(a) and ia * len(b) <= ib * len(a)):
                    out.append(a[ia]); ia += 1
                else:
                    out.append(b[ib]); ib += 1
            return out

        nxt = gla_tile(hp, order[0][0], order[0][1])
        for i, (j, own) in enumerate(order):
            cur = nxt
            pre_ops = []
            if i + 1 < len(order):
                pre_ops, nxt = capture(lambda: gla_tile(hp, order[i + 1][0], order[i + 1][1]))
            if own and j == 0:
                P.add("dve", lambda e: e.tensor_scalar(out=S_f.rearrange("p a b -> p (a b)"), in0=S_f.rearrange("p a b -> p (a b)"), scalar1=flagcol, scalar2=None, op0=ALU.mult),
                      reads=["cB", ("S_f", 0), ("S_f", 1)], writes=[("S_f", 0), ("S_f", 1)])
                P.add("act", lambda e: e.activation(out=S_bf.rearrange("p a b -> p (a b)"), in_=S_f.rearrange("p a b -> p (a b)"), func=AF.Copy),
                      reads=[("S_f", 0), ("S_f", 1)], writes=[("S_bf", 0), ("S_bf", 1)])
            head_ops, _ = capture(cur)
            P.ops.extend(merge(pre_ops, head_ops))

    gblocks = [("ga", C_GA, 16, None, None)]
    for hp in range(2):
        gblocks.append(("gq", C_GQ + hp * 256, 256, OWN, hp))
        gblocks.append(("gk", C_GK + hp * 256, 256, ALL, hp))
        gblocks.append(("gv", C_GV + hp * 512, 512, ALL, hp))
        gblocks.append(("gr", C_GR + hp * 512, 512, OWN, hp))
    cons = dict(gq=copy_consumer(gqb, "gq", 256), gk=copy_consumer(gkb, "gk", 256), gv=copy_consumer(gvb, "gv", 512), gr=copy_consumer(grb, "gr", 512))
    gslots = [None] * len(gblocks)
    gslots[0] = load_w(gblocks[0][1], gblocks[0][2])
    for bi, (kind, c0, n, tiles, hp) in enumerate(gblocks):
        if bi + 1 < len(gblocks):
            gslots[bi + 1] = load_w(gblocks[bi + 1][1], gblocks[bi + 1][2])
        if kind == "ga":
            ga_block(gslots[bi])
            continue
        for j in tiles:
            b = proj_tile(gslots[bi], n, j)
            cons[kind](j, b)
        if kind == "gr":
            gla_scan(hp)

    if stage == "gla":
        P.add("sp", lambda e: e.dma_start(out=g["dbgb_d"][:, :], in_=mixT.rearrange("p a b -> p (a b)")),
              reads=[("mixT", h, q) for h in range(8) for q in range(2)] + [("mixTg", 8 + 2 * h, j) for h in range(4) for j in range(NTO)],
              writes=["dbg"], dma=K("dbg"))
        return

    RA, RB, RC, RE = g["RA"], g["RB"], g["RC"], g["RE"]
    w_out_v, x_own, x1_d, xg_d, yb_d, out_d, g2_d = (g[k] for k in ("w_out_v", "x_own", "x1_d", "xg_d", "yb_d", "out_d", "g2_d"))
    trs_bf, wr_bf = g["trs_bf"], g["wr_bf"]
    wout = hT
    mix_res = [("mixT", h, q) for h in range(8) for q in range(2)] + [("mixTg", 8 + 2 * h, j) for h in range(4) for j in range(NTO)]
    for cb in range(4):
        P.add("pool", (lambda cb: lambda e: e.dma_start(out=wout[:, :, cb * 512:(cb + 1) * 512], in_=w_out_v[:, :, cb * 512:(cb + 1) * 512]))(cb),
              writes=[("hT", j) for j in ALL] + [("wout", cb)], dma=K(("wout", cb)), nophase=True)
    pos1s = RE.alloc(NTO * 4, I32)
    pos2s = RE.alloc(NTO * 4, I32)
    pos1g = RE.alloc(NTO * 4, I32)
    pos2g = RE.alloc(NTO * 4, I32)
    cw1 = RE.alloc(NTO * 4, F32)
    cw2 = RE.alloc(NTO * 4, F32)
    P.barrier(bar)
    RD.reset()
    xr = [RD.alloc(DM * 4, F32) for _ in range(2)]
    x1 = [RD.alloc(DM * 4, F32) for _ in range(2)]
    g2 = RD.alloc(DM * 4, F32)
    h2b = [RD.alloc(DM * 2, BF16) for _ in range(2)]
    h2T = RD.alloc(KC * 128 * 2, BF16, [KC, 128])
    sq2 = RD.alloc(DM * 2, BF16)
    Rb = RD.alloc(NTO * 32 * 2, BF16, [NTO, 32])
    ss2 = RD.alloc(4, F32)
    rs2 = RD.alloc(4, F32)
    lg = RD.alloc(36 * 4, F32)
    W = {}
    for nm, n in (("mxg", 1), ("ohg", 4), ("negm", 1), ("eg4", 4), ("sg", 1), ("pg", 1), ("tmp", 32), ("les", 8), ("m1", 1), ("oh1", 8),
                  ("msk", 8), ("m2", 1), ("oh2", 8), ("dm", 1), ("ed", 1), ("w1", 1), ("w2", 1), ("R1", 32), ("R2", 32), ("pr", 32),
                  ("vl", 32), ("t32", 32), ("p1", 1), ("v1", 1), ("p2", 1), ("v2", 1), ("tA", 1), ("tB", 1)):
        W[nm] = RD.alloc(n * 4, F32)
    rbias = cB[:, CB_RB:CB_RB + 36]
    pbase = cB[:, CB_PB:CB_PB + 32]
    P.add("sp", lambda e: e.dma_start(out=g2, in_=g2_d[:, :]), writes=["g2"], dma=K("g2"))
    emit_conv(100)
    XGD = [("xgd", u_) for u_ in range(NE * CAP // 128)]
    BIGI = 20000.0

    def D(fn, reads, writes):
        P.add("dve", fn, reads=reads, writes=writes)

    def outproj_tile(j):
        pj = j % 2
        rsl = slice(j * 128, (j + 1) * 128)
        P.add("sp", lambda e: e.dma_start(out=xr[pj], in_=x_own[rsl, :]), writes=[("xr", pj)], dma=K(("xr", pj)))
        for cb in range(4):
            b = mmbank[0] % 2
            mmbank[0] += 1
            csl = slice(cb * 512, (cb + 1) * 512)
            for kc in range(KC):
                P.add("pe", (lambda kc, b, csl: lambda e: e.matmul(ps[b][:, :], lhsT=mixT[:, kc, rsl], rhs=wout[:, kc, csl], start=(kc == 0), stop=(kc == KC - 1)))(kc, b, csl),
                      reads=mix_res + [("wout", cb)], writes=[PSR(b)])
            P.add("dve", (lambda b, csl: lambda e: e.tensor_tensor(out=x1[pj][:, csl], in0=ps[b][:, :], in1=xr[pj][:, csl], op=ALU.add))(b, csl),
                  reads=[("xr", pj)], writes=[PSR(b), ("x1", pj, cb)])

    def dispatch_tile(j):
        pj = j % 2
        rsl = slice(j * 128, (j + 1) * 128)
        x1r = [("x1", pj, cb) for cb in range(4)]
        P.add("sp", lambda e: e.dma_start(out=x1_d[rsl, :], in_=x1[pj]), reads=x1r, writes=[("x1d", j)], dma=K(("x1s", pj)))
        P.add("act", lambda e: e.activation(out=sq2, in_=x1[pj], func=AF.Square, accum_out=ss2), reads=x1r, writes=["sq2", "ss2"])
        P.add("act", lambda e: e.activation(out=rs2, in_=ss2, func=AF.Ln, scale=1.0 / DM, bias=EPS), reads=["ss2"], writes=["rs2"])
        P.add("act", lambda e: e.activation(out=rs2, in_=rs2, func=AF.Exp, scale=-0.5), reads=["rs2"], writes=["rs2"])
        D(lambda e: e.scalar_tensor_tensor(out=h2b[pj], in0=x1[pj], scalar=rs2, in1=g2, op0=ALU.mult, op1=ALU.mult), x1r + ["rs2", "g2"], [("h2b", pj)])
        for half in range(2):
            bank = 2 + half
            for i in range(8):
                kc = half * 8 + i
                P.add("pe", (lambda kc, i, bank: lambda e: e.transpose(out=psb[bank][:, i * 128:(i + 1) * 128], in_=h2b[pj][:, kc * 128:(kc + 1) * 128],
                                                                       identity=ident_bf))(kc, i, bank), reads=[("h2b", pj), "ident"], writes=[PSR(bank)])
            if half == 0:
                P.add("act", lambda e: e.activation(out=h2T[:, 0:8, :], in_=psb[2].rearrange("p (a b) -> p a b", b=128), func=AF.Copy), reads=[], writes=[PSR(2), "h2Ta"])
            else:
                D(lambda e: e.tensor_copy(out=h2T[:, 8:16, :], in_=psb[3].rearrange("p (a b) -> p a b", b=128)), [], [PSR(3), "h2Tb"])
        for kc in range(KC):
            P.add("pe", (lambda kc: lambda e: e.matmul(ps[4][:, 0:36], lhsT=h2T[:, kc, :], rhs=wr_bf[:, kc, :], start=(kc == 0), stop=(kc == KC - 1)))(kc),
                  reads=["h2Ta", "h2Tb", "wr"], writes=[PSR(4)])
        D(lambda e: e.tensor_tensor(out=lg, in0=ps[4][:, 0:36], in1=rbias, op=ALU.add), ["cB"], [PSR(4), "lg"])
        lgg = lg[:, 0:4]
        le = lg[:, 4:36].rearrange("p (a b) -> p a b", b=8)
        D(lambda e: e.tensor_reduce(out=W["mxg"], in_=lgg, axis=AX.X, op=ALU.max), ["lg"], ["mxg"])
        D(lambda e: e.tensor_scalar(out=W["ohg"], in0=lgg, scalar1=W["mxg"], scalar2=None, op0=ALU.is_equal), ["lg", "mxg"], ["ohg"])
        D(lambda e: e.tensor_scalar_mul(out=W["negm"], in0=W["mxg"], scalar1=-1.0), ["mxg"], ["negm"])
        P.add("act", lambda e: e.activation(out=W["eg4"], in_=lgg, func=AF.Exp, bias=W["negm"], accum_out=W["sg"]), reads=["lg", "negm"], writes=["eg4", "sg"])
        D(lambda e: e.reciprocal(out=W["pg"], in_=W["sg"]), ["sg"], ["pg"])
        t3 = W["tmp"].rearrange("p (a b) -> p a b", b=8)
        D(lambda e: e.tensor_tensor(out=t3, in0=le, in1=W["ohg"].unsqueeze(2).to_broadcast([128, 4, 8]), op=ALU.mult), ["lg", "ohg"], ["tmp"])
        D(lambda e: e.tensor_reduce(out=W["les"], in_=t3.rearrange("p a b -> p b a"), axis=AX.X, op=ALU.add), ["tmp"], ["les"])
        D(lambda e: e.tensor_reduce(out=W["m1"], in_=W["les"], axis=AX.X, op=ALU.max), ["les"], ["m1"])
        D(lambda e: e.tensor_scalar(out=W["oh1"], in0=W["les"], scalar1=W["m1"], scalar2=None, op0=ALU.is_equal), ["les", "m1"], ["oh1"])
        D(lambda e: e.scalar_tensor_tensor(out=W["msk"], in0=W["oh1"], scalar=-1e30, in1=W["les"], op0=ALU.mult, op1=ALU.add), ["oh1", "les"], ["msk"])
        D(lambda e: e.tensor_reduce(out=W["m2"], in_=W["msk"], axis=AX.X, op=ALU.max), ["msk"], ["m2"])
        D(lambda e: e.tensor_scalar(out=W["oh2"], in0=W["msk"], scalar1=W["m2"], scalar2=None, op0=ALU.is_equal), ["msk", "m2"], ["oh2"])
        D(lambda e: e.tensor_tensor(out=W["dm"], in0=W["m2"], in1=W["m1"], op=ALU.subtract), ["m1", "m2"], ["dm"])
        P.add("act", lambda e: e.activation(out=W["ed"], in_=W["dm"], func=AF.Exp), reads=["dm"], writes=["ed"])
        D(lambda e: e.tensor_scalar_add(out=W["w1"], in0=W["ed"], scalar1=1.0), ["ed"], ["w1"])
        D(lambda e: e.reciprocal(out=W["w1"], in_=W["w1"]), ["w1"], ["w1"])
        D(lambda e: e.tensor_tensor(out=W["w2"], in0=W["ed"], in1=W["w1"], op=ALU.mult), ["ed", "w1"], ["w2"])
        R1 = W["R1"].rearrange("p (a b) -> p a b", b=8)
        R2 = W["R2"].rearrange("p (a b) -> p a b", b=8)
        ohgb = W["ohg"].unsqueeze(2).to_broadcast([128, 4, 8])
        D(lambda e: e.tensor_tensor(out=R1, in0=ohgb, in1=W["oh1"].unsqueeze(1).to_broadcast([128, 4, 8]), op=ALU.mult), ["ohg", "oh1"], ["R1"])
        D(lambda e: e.tensor_tensor(out=R2, in0=ohgb, in1=W["oh2"].unsqueeze(1).to_broadcast([128, 4, 8]), op=ALU.mult), ["ohg", "oh2"], ["R2"])
        D(lambda e: e.tensor_tensor(out=Rb[:, j, :], in0=W["R1"], in1=W["R2"], op=ALU.add), ["R1", "R2"], [("Rb", j)])
        P.add("pe", lambda e: e.matmul(ps[5][:, 0:32], lhsT=trs_bf, rhs=Rb[:, j, :], start=True, stop=(j == 0)), reads=["trs", ("Rb", j)], writes=[PSR(5)])
        for i in range(j):
            P.add("pe", (lambda i: lambda e: e.matmul(ps[5][:, 0:32], lhsT=ones_bf, rhs=Rb[:, i, :], start=False, stop=(i == j - 1)))(i),
                  reads=["ones", ("Rb", i)], writes=[PSR(5)])
        D(lambda e: e.tensor_tensor(out=W["pr"], in0=ps[5][:, 0:32], in1=pbase, op=ALU.add), ["cB"], [PSR(5), "pr"])
        D(lambda e: e.tensor_single_scalar(out=W["vl"], in_=ps[5][:, 0:32], scalar=float(CAP), op=ALU.is_lt), [], [PSR(5), "vl"])
        for (Rk, pk, vk, cwk, posS, posG, wk) in (("R1", "p1", "v1", cw1, pos1s, pos1g, "w1"), ("R2", "p2", "v2", cw2, pos2s, pos2g, "w2")):
            def one(Rk=Rk, pk=pk, vk=vk, cwk=cwk, posS=posS, posG=posG, wk=wk):
                D(lambda e: e.tensor_tensor(out=W["t32"], in0=W[Rk], in1=W["pr"], op=ALU.mult), [Rk, "pr"], ["t32"])
                D(lambda e: e.tensor_reduce(out=W[pk], in_=W["t32"], axis=AX.X, op=ALU.add), ["t32"], [pk])
                D(lambda e: e.tensor_tensor(out=W["t32"], in0=W[Rk], in1=W["vl"], op=ALU.mult), [Rk, "vl", pk], ["t32"])
                D(lambda e: e.tensor_reduce(out=W[vk], in_=W["t32"], axis=AX.X, op=ALU.add), ["t32"], [vk])
                D(lambda e: e.tensor_scalar_add(out=W["tA"], in0=W[pk], scalar1=-BIGI), [pk], ["tA"])
                D(lambda e: e.tensor_tensor(out=W["tA"], in0=W["tA"], in1=W[vk], op=ALU.mult), ["tA", vk], ["tA"])
                D(lambda e: e.tensor_scalar_add(out=W["tA"], in0=W["tA"], scalar1=BIGI), ["tA"], ["tA"])
                D(lambda e: e.tensor_copy(out=posS[:, j:j + 1], in_=W["tA"]), ["tA"], [("posS", pk, j)])
                D(lambda e: e.tensor_tensor(out=W["tB"], in0=W[pk], in1=W[vk], op=ALU.mult), [pk, vk], ["tB"])
                D(lambda e: e.tensor_copy(out=posG[:, j:j + 1], in_=W["tB"]), ["tB"], [("posG", pk, j)])
                D(lambda e: e.tensor_tensor(out=W["tB"], in0=W[wk], in1=W[vk], op=ALU.mult), [wk, vk, ("posG", pk, j)], ["tB"])
                D(lambda e: e.tensor_tensor(out=cwk[:, j:j + 1], in0=W["tB"], in1=W["pg"], op=ALU.mult), ["tB", "pg"], [("cw", pk, j)])
                P.add("pool", lambda e: e.indirect_dma_start(out=xg_d[:, :], out_offset=bass.IndirectOffsetOnAxis(ap=posS[:, j:j + 1], axis=0),
                                                             in_=h2b[pj][:, :], in_offset=None, bounds_check=NE * CAP - 1, oob_is_err=False),
                      reads=[("h2b", pj), ("posS", pk, j)], writes=XGD, dma=K(("sc", pj, pk)))
            one()

    outproj_tile(0)
    for j in range(NTO):
        a_ops = []
        if j + 1 < NTO:
            a_ops, _ = P.capture(lambda: outproj_tile(j + 1))
        b_ops, _ = P.capture(lambda: dispatch_tile(j))
        P.ops.extend(Prog.merge(a_ops, b_ops))

    if stage == "disp":
        RC.reset()
        d1 = RC.alloc(4096, F32)
        P.add("dve", lambda e: e.tensor_copy(out=d1[:, 0:8], in_=pos1s), reads=[("posS", "p1", j) for j in range(NTO)], writes=["d1a"])
        P.add("dve", lambda e: e.tensor_copy(out=d1[:, 8:16], in_=pos2s), reads=[("posS", "p2", j) for j in range(NTO)], writes=["d1b"])
        P.add("dve", lambda e: e.tensor_copy(out=d1[:, 16:24], in_=cw1), reads=[("cw", "p1", j) for j in range(NTO)], writes=["d1c"])
        P.add("dve", lambda e: e.tensor_copy(out=d1[:, 24:32], in_=cw2), reads=[("cw", "p2", j) for j in range(NTO)], writes=["d1d"])
        P.add("sp", lambda e: e.dma_start(out=g["dbg_d"][:, 2048:4096], in_=x1[1]), reads=[("x1", 1, cb) for cb in range(4)], writes=["dbgx"], dma=K("dbgx"))
        P.add("sp", lambda e: e.dma_start(out=g["dbg_d"][:, 4096:4132], in_=lg), reads=["lg"], writes=["dbgy"], dma=K("dbgy"))
        P.add("sp", lambda e: e.dma_start(out=g["dbgb_d"][:, 0:2048], in_=h2b[1]), reads=[("h2b", 1)], writes=["dbgz"], dma=K("dbgz"))
        P.add("sp", lambda e: e.dma_start(out=g["dbgb_d"][:, 2048:4096], in_=wout[:, 3, :]), reads=[("wout", cb) for cb in range(4)], writes=["dbgw"], dma=K("dbgw"))
        P.add("sp", lambda e: e.dma_start(out=g["dbgb_d"][:, 4096:5120], in_=mixT[:, 9, :]), reads=mix_res, writes=["dbgm"], dma=K("dbgm"))
        P.add("sp", lambda e: e.dma_start(out=g["dbg_d"][:, 4200:4200 + 2048], in_=xr[1]), reads=[("xr", 1)], writes=["dbgr"], dma=K("dbgr"))
        P.add("sp", lambda e: e.dma_start(out=g["dbg_d"][:, 0:32], in_=d1[:, 0:32]), reads=["dbgx", "dbgy", "dbgz", "dbgw", "dbgm", "dbgr", "d1a", "d1b", "d1c", "d1d"] + [("x1d", j) for j in range(NTO)] + XGD,
              writes=["dbg"], dma=K("dbg"))
        return

    w_gate, w_up, w_down = g["w_gate"], g["w_up"], g["w_down"]
    P.barrier(bar)
    RD.reset()
    RA.reset()
    big = RA.big
    wslots = []
    for s_ in range(2):
        base = s_ * 48 * 1024
        wg_ = big[:, base:base + 16384].bitcast(BF16).rearrange("p (a b) -> p a b", b=FF)
        wu_ = big[:, base + 16384:base + 32768].bitcast(BF16).rearrange("p (a b) -> p a b", b=FF)
        wd_ = big[:, base + 32768:base + 49152].bitcast(BF16).rearrange("p (a b) -> p a b", b=DM)
        wslots.append((wg_, wu_, wd_))
    xg = [RD.alloc(DM * 2, BF16) for _ in range(2)]
    xgT = [RD.alloc(KC * 128 * 2, BF16, [KC, 128]) for _ in range(2)]
    sgt = RD.alloc(FF * 4, F32)
    hg = RD.alloc(FF * 2, BF16)
    hgT = RD.alloc(4 * 128 * 2, BF16, [4, 128])
    Ye = [RD.alloc(DM * 2, BF16) for _ in range(2)]
    old_w = [("wout", cb) for cb in range(4)] + [("wb", 0), ("wb", 1)] + [("hT", j) for j in ALL]

    NSTG = 8
    RC.reset()
    stg = [RC.alloc(DM * 4, F32) for _ in range(4)] + [RD.alloc(DM * 4, F32) for _ in range(4)]
    NCHUNK = NE * 12

    NCONV, wcb_d, CONV_SET = g["NCONV"], g["wcb_d"], g["CONV_SET"]
    items = []
    for e_ in range(NE):
        s_ = e_ % 2
        wg_, wu_, wd_ = wslots[s_]
        if e_ in CONV_SET:
            m0 = 3 * CONV_SET.index(e_)
            items.append(dict(kind="direct", e=e_, src=wcb_d[m0].rearrange("p (a b) -> p a b", b=FF), dst=wg_, res=[("wg", s_, c) for c in range(4)], conv=m0))
            items.append(dict(kind="direct", e=e_, src=wcb_d[m0 + 1].rearrange("p (a b) -> p a b", b=FF), dst=wu_, res=[("wu", s_, c) for c in range(4)], conv=m0 + 1))
            items.append(dict(kind="direct", e=e_, src=wcb_d[m0 + 2].rearrange("p (a b) -> p a b", b=DM), dst=wd_, res=[("wd", s_, c) for c in range(4)], conv=m0 + 2))
        else:
            for c in range(4):
                items.append(dict(kind="staged", e=e_, src=w_gate[e_].rearrange("(kc p) f -> p kc f", p=128)[:, 4 * c:4 * c + 4, :], dst=wg_[:, 4 * c:4 * c + 4, :], res=[("wg", s_, c)], is3=True))
            for c in range(4):
                items.append(dict(kind="staged", e=e_, src=w_up[e_].rearrange("(kc p) f -> p kc f", p=128)[:, 4 * c:4 * c + 4, :], dst=wu_[:, 4 * c:4 * c + 4, :], res=[("wu", s_, c)], is3=True))
            for c in range(4):
                items.append(dict(kind="staged", e=e_, src=w_down[e_].rearrange("(fc p) d -> p fc d", p=128)[:, c, :], dst=wd_[:, c, :], res=[("wd", s_, c)], is3=False))
    st_ = dict(nd=0, ring_used=0, nstaged=0, ncast=0)

    def pump(max_e):
        while st_["nd"] < len(items):
            it = items[st_["nd"]]
            if it["e"] > max_e or (it["kind"] == "direct" and it["e"] > max_e - 1):
                return
            if it["kind"] == "staged":
                if st_["ring_used"] >= NSTG:
                    return
                k = st_["nstaged"] % NSTG
                st_["nstaged"] += 1
                st_["ring_used"] += 1
                it["slot"] = k
                sv = stg[k].rearrange("p (a b) -> p a b", b=FF) if it["is3"] else stg[k]
                it["sv"] = sv
                P.add("sp", (lambda sv, src: lambda e: e.dma_start(out=sv, in_=src))(sv, it["src"]), writes=[("stg", k)], dma=K(("stg", k)))
            else:
                early = False
                P.add("sp", (lambda dst, src: lambda e: e.dma_start(out=dst, in_=src))(it["dst"], it["src"]), reads=[("conv", it["conv"]), ("conv", 3 * NCONV - 1)],
                      writes=it["res"] + (old_w if early else []), dma=K(("wdir", it["e"] % 2, it["conv"] % 3)), nophase=early)
            st_["nd"] += 1

    def cast_items(e_):
        pump(e_ + 1)
        for it in items:
            if it["e"] != e_ or it["kind"] != "staged":
                continue
            k, sv, dst, res = it["slot"], it["sv"], it["dst"], it["res"]
            if st_["ncast"] % 2 == 0:
                P.add("act", (lambda dst, sv: lambda e: e.activation(out=dst, in_=sv, func=AF.Copy))(dst, sv), reads=[("stg", k)], writes=res)
            else:
                P.add("dve", (lambda dst, sv: lambda e: e.tensor_copy(out=dst, in_=sv))(dst, sv), reads=[("stg", k)], writes=res)
            st_["ncast"] += 1
            st_["ring_used"] -= 1
            pump(e_ + 1)

    def load_xg(e_, st, u):
        pu = u % 2
        r0 = e_ * CAP + st * 128
        P.add("act", lambda e: e.dma_start(out=xg[pu], in_=xg_d[r0:r0 + 128, :]), reads=XGD, writes=[("xg", pu)], dma=K(("xg", pu)))

    def expert_unit(e_, st, u, part):
        s_ = e_ % 2
        pu = u % 2
        wg_, wu_, wd_ = wslots[s_]
        r0 = e_ * CAP + st * 128
        if part == "front":
            unit_front(s_, pu, wg_, wu_)
        elif part == "b1":
            P.add("act", lambda e: e.activation(out=sgt, in_=ps[0][:, :], func=AF.Silu), reads=[], writes=[PSR(0), "sgt"])
            P.add("dve", lambda e: e.tensor_tensor(out=hg, in0=ps[1][:, :], in1=sgt, op=ALU.mult), reads=["sgt"], writes=[PSR(1), "hg"])
        else:
            unit_back(s_, pu, wd_, r0, u)

    def unit_front(s_, pu, wg_, wu_):
        for half in range(2):
            bank = 2 + half
            for i in range(8):
                kc = half * 8 + i
                P.add("pe", (lambda kc, i, bank: lambda e: e.transpose(out=psb[bank][:, i * 128:(i + 1) * 128], in_=xg[pu][:, kc * 128:(kc + 1) * 128],
                                                                       identity=ident_bf))(kc, i, bank), reads=[("xg", pu), "ident"], writes=[PSR(bank)])
            if half == 0:
                P.add("act", lambda e: e.activation(out=xgT[pu][:, 0:8, :], in_=psb[2].rearrange("p (a b) -> p a b", b=128), func=AF.Copy),
                      reads=[], writes=[PSR(2), ("xgTa", pu)])
            else:
                P.add("dve", lambda e: e.tensor_copy(out=xgT[pu][:, 8:16, :], in_=psb[3].rearrange("p (a b) -> p a b", b=128)),
                      reads=[], writes=[PSR(3), ("xgTb", pu)])
        for kc in range(KC):
            P.add("pe", (lambda kc: lambda e: e.matmul(ps[0][:, :], lhsT=xgT[pu][:, kc, :], rhs=wg_[:, kc, :], start=(kc == 0), stop=(kc == KC - 1)))(kc),
                  reads=[("xgTa", pu), ("xgTb", pu), ("wg", s_, kc // 4)], writes=[PSR(0)])
        for kc in range(KC):
            P.add("pe", (lambda kc: lambda e: e.matmul(ps[1][:, :], lhsT=xgT[pu][:, kc, :], rhs=wu_[:, kc, :], start=(kc == 0), stop=(kc == KC - 1)))(kc),
                  reads=[("xgTa", pu), ("xgTb", pu), ("wu", s_, kc // 4)], writes=[PSR(1)])

    def unit_back(s_, pu, wd_, r0, u):
        for fc in range(4):
            P.add("pe", (lambda fc: lambda e: e.transpose(out=psb[4][:, fc * 128:(fc + 1) * 128], in_=hg[:, fc * 128:(fc + 1) * 128], identity=ident_bf))(fc),
                  reads=["hg", "ident"], writes=[PSR(4)])
        P.add("act", lambda e: e.activation(out=hgT, in_=psb[4][:, 0:512].rearrange("p (a b) -> p a b", b=128), func=AF.Copy), reads=[], writes=[PSR(4), "hgT"])
        for db in range(4):
            bank = 5 + db % 2
            for fc in range(4):
                P.add("pe", (lambda fc, db, bank: lambda e: e.matmul(ps[bank][:, :], lhsT=hgT[:, fc, :], rhs=wd_[:, fc, db * 512:(db + 1) * 512],
                                                                     start=(fc == 0), stop=(fc == 3)))(fc, db, bank), reads=["hgT", ("wd", s_, fc)], writes=[PSR(bank)])
            if db % 2 == 0:
                P.add("act", (lambda db, bank: lambda e: e.activation(out=Ye[pu][:, db * 512:(db + 1) * 512], in_=ps[bank][:, :], func=AF.Copy))(db, bank),
                      reads=[], writes=[PSR(bank), ("Ye", pu, db)])
            else:
                P.add("dve", (lambda db, bank: lambda e: e.tensor_copy(out=Ye[pu][:, db * 512:(db + 1) * 512], in_=ps[bank][:, :]))(db, bank),
                      reads=[], writes=[PSR(bank), ("Ye", pu, db)])
        P.add("act", lambda e: e.dma_start(out=yb_d[r0:r0 + 128, :], in_=Ye[pu]), reads=[("Ye", pu, db) for db in range(4)], writes=[("yd", u)], dma=K(("ye", pu)))

    NST = CAP // 128
    cast_items(0)
    units = [(e_, st) for e_ in range(NE) for st in range(NST)]
    NU = len(units)
    assert NST == 2
    load_xg(units[0][0], units[0][1], 0)
    load_xg(units[1][0], units[1][1], 1)
    expert_unit(units[0][0], units[0][1], 0, "front")

    def back_and_next_front(u):
        a_ops, _ = P.capture(lambda: expert_unit(units[u][0], units[u][1], u, "b23"))
        b_ops = []
        if u + 1 < NU:
            def nf():
                if u + 2 < NU:
                    load_xg(units[u + 2][0], units[u + 2][1], u + 2)
                expert_unit(units[u + 1][0], units[u + 1][1], u + 1, "front")
            b_ops, _ = P.capture(nf)
        P.ops.extend(Prog.merge(a_ops, b_ops))

    for e_ in range(NE):
        u0 = 2 * e_

        def part1():
            expert_unit(e_, 0, u0, "b1")
            back_and_next_front(u0)
            expert_unit(e_, 1, u0 + 1, "b1")

        def cast_stream():
            if e_ + 1 < NE:
                cast_items(e_ + 1)
        a_ops, _ = P.capture(part1)
        b_ops, _ = P.capture(cast_stream)
        P.ops.extend(Prog.merge(a_ops, b_ops))
        back_and_next_front(u0 + 1)
    u = NU
    YD = [("yd", i) for i in range(u)]

    P.barrier(bar)
    RC.reset()
    y1b = [RC.alloc(DM * 2, BF16) for _ in range(2)]
    y2b = [RC.alloc(DM * 2, BF16) for _ in range(2)]
    xfb = [RC.alloc(DM * 4, F32) for _ in range(2)]

    def final_tile(j):
        rsl = slice(j * 128, (j + 1) * 128)
        fj = j % 2
        y1, y2, xf = y1b[fj], y2b[fj], xfb[fj]
        P.add("pool", lambda e: e.indirect_dma_start(out=y1[:, :], out_offset=None, in_=yb_d[:, :],
                                                     in_offset=bass.IndirectOffsetOnAxis(ap=pos1g[:, j:j + 1], axis=0),
                                                     bounds_check=NE * CAP - 1, oob_is_err=False),
              reads=YD + [("posG", "p1", j)] + mix_res, writes=[("y1", fj)], dma=K(("y1", fj)))
        P.add("pool", lambda e: e.indirect_dma_start(out=y2[:, :], out_offset=None, in_=yb_d[:, :],
                                                     in_offset=bass.IndirectOffsetOnAxis(ap=pos2g[:, j:j + 1], axis=0),
                                                     bounds_check=NE * CAP - 1, oob_is_err=False),
              reads=YD + [("posG", "p2", j)] + mix_res, writes=[("y2", fj)], dma=K(("y2", fj)))
        P.add("sp", lambda e: e.dma_start(out=xf, in_=x1_d[rsl, :]), reads=[("x1d", j)] + mix_res, writes=[("xf", fj)], dma=K(("xf", fj)))
        P.add("dve", lambda e: e.scalar_tensor_tensor(out=xf, in0=y1, scalar=cw1[:, j:j + 1], in1=xf, op0=ALU.mult, op1=ALU.add),
              reads=[("y1", fj), ("xf", fj), ("cw", "p1", j)], writes=[("xf", fj)])
        P.add("dve", lambda e: e.scalar_tensor_tensor(out=xf, in0=y2, scalar=cw2[:, j:j + 1], in1=xf, op0=ALU.mult, op1=ALU.add),
              reads=[("y2", fj), ("xf", fj), ("cw", "p2", j)], writes=[("xf", fj)])
        P.add("sp", lambda e: e.dma_start(out=out_d[rsl, :], in_=xf), reads=[("xf", fj)], writes=[("outd", j)], dma=K(("outd", fj)))

    for j in range(NTO):
        final_tile(j)
    g["final_reads"].extend([("outd", j) for j in range(NTO)])
```
